# Optimizing a Trainium2 kernel written in Bass

```python
import math
import jax
import jax.numpy as jnp
from jax import lax
import numpy as np

D_MODEL = 2048
BATCH = 4
SEQ = 2048
DEPTH = 1

GRID_W = 64
CTX_LEN = 256
EPS = 1e-6
N_MOD = 6

S5_WIDTH = D_MODEL // 2
S5_GROUP = 16
S5_GROUPS = S5_WIDTH // S5_GROUP
S5_STATE = 64
S5_DT_MIN = 0.001
S5_DT_MAX = 0.1

GLA_HEADS = 4
GLA_KEY_WIDTH = D_MODEL // 4
GLA_VAL_WIDTH = D_MODEL // 2
GLA_DK = GLA_KEY_WIDTH // GLA_HEADS
GLA_DV = GLA_VAL_WIDTH // GLA_HEADS
GLA_GATE_RANK = 16
GLA_GATE_NORM = 16.0
GLA_CHUNK = 64

IN_SPLITS = (S5_WIDTH, GLA_KEY_WIDTH, GLA_KEY_WIDTH, GLA_VAL_WIDTH, GLA_VAL_WIDTH, 2 * GLA_GATE_RANK, D_MODEL, D_MODEL)
IN_WIDTH = sum(IN_SPLITS)

N_EXPERTS = 64
TOP_K = 8
N_GROUPS = 8
TOPK_GROUPS = 4
D_EXPERT = D_MODEL // 4
D_SHARED = D_MODEL // 4
ROUTED_SCALE = 2.5
EXPERT_BLOCK = 128

kernel_name = 'hybrid_s5_gla_moe_diffusion_block'


def _flip(t):
    return jnp.flip(t, axis=1)


def _ident(t):
    return t


def rmsnorm(x, w):
    xf = x.astype(jnp.float32)
    y = xf * lax.rsqrt(jnp.mean(xf * xf, axis=-1, keepdims=True) + EPS)
    return (y * w.astype(jnp.float32)).astype(x.dtype)


def modulate(x, w, shift, scale):
    return rmsnorm(x, w) * (1.0 + scale) + shift


def s5_discretize(lam_re, lam_im, log_dt, b_re, b_im):
    lam = lax.complex(lam_re.astype(jnp.float32), lam_im.astype(jnp.float32))
    dt = jnp.exp(log_dt.astype(jnp.float32))[:, None]
    lam_bar = jnp.exp(lam * dt)
    b = lax.complex(b_re.astype(jnp.float32), b_im.astype(jnp.float32))
    b_bar = ((lam_bar - 1.0) / lam)[..., None] * b
    return lam_bar, b_bar


def _linear_recurrence(e1, e2):
    a1, b1 = e1
    a2, b2 = e2
    return a1 * a2, a2 * b1 + b2


def s5_scan(u, lam_bar, b_bar, h0):
    bu = jnp.einsum('blgh,gph->blgp', u.astype(jnp.complex64), b_bar)
    if h0 is not None:
        bu = bu.at[:, 0].add(lam_bar * h0)
    a = jnp.broadcast_to(lam_bar, bu.shape)
    _, hs = lax.associative_scan(_linear_recurrence, (a, bu), axis=1)
    return hs


def s5_readout(hs, c_mat):
    return jnp.einsum('blgp,ghp->blgh', hs, c_mat).real


def s5_bidir(u, uc, lam_re, lam_im, log_dt, b_re, b_im, c_re, c_im, d_skip, ctx_out):
    def grp(t):
        return t.astype(jnp.float32).reshape(t.shape[0], t.shape[1], S5_GROUPS, S5_GROUP)
    ug, ucg = grp(u), grp(uc)
    dsk = d_skip.astype(jnp.float32).reshape(S5_GROUPS, S5_GROUP)
    y = dsk * ug
    yc = dsk * ucg if ctx_out else None
    for dr in range(2):
        flip = _flip if dr else _ident
        lam_bar, b_bar = s5_discretize(lam_re[dr], lam_im[dr], log_dt[dr], b_re[dr], b_im[dr])
        c_mat = lax.complex(c_re[dr].astype(jnp.float32), c_im[dr].astype(jnp.float32))
        hs_c = s5_scan(flip(ucg), lam_bar, b_bar, None)
        hs = s5_scan(flip(ug), lam_bar, b_bar, hs_c[:, -1])
        y = y + flip(s5_readout(hs, c_mat))
        if ctx_out:
            yc = yc + flip(s5_readout(hs_c, c_mat))
    y = y.reshape(u.shape).astype(u.dtype)
    if ctx_out:
        yc = yc.reshape(uc.shape).astype(uc.dtype)
    return y, yc


def s5_glu(y, w):
    y = jax.nn.gelu(y)
    return y * jax.nn.sigmoid(y @ w)


def gla_states(k, v, b, s0):
    b_last = b[:, :, :, -1:, :]
    ds = jnp.einsum('bhnck,bhncv->bhnkv', k * jnp.exp(b_last - b), v)
    decay = jnp.exp(b_last[:, :, :, 0, :])

    def step(s, xs):
        d, dsn = xs
        return d[..., None] * s + dsn, s

    s_final, starts = lax.scan(step, s0, (jnp.moveaxis(decay, 2, 0), jnp.moveaxis(ds, 2, 0)))
    return jnp.moveaxis(starts, 0, 2), s_final


def gla_readout(q, k, v, b, starts, strict):
    qb = q * jnp.exp(b)
    kb = k * jnp.exp(-b)
    scores = jnp.einsum('bhnik,bhnjk->bhnij', qb, kb)
    mask = jnp.tril(jnp.ones((GLA_CHUNK, GLA_CHUNK), bool), k=-1 if strict else 0)
    intra = jnp.einsum('bhnij,bhnjv->bhniv', jnp.where(mask, scores, 0.0), v)
    inter = jnp.einsum('bhnik,bhnkv->bhniv', qb, starts)
    return intra + inter


def gla_bidir(q, k, v, gkd, qc, kc, vc, gkdc, gk_up, gk_b, ctx_out):
    f32 = jnp.float32

    def chunks(t, d, flip):
        t = flip(t.astype(f32))
        b_, l_ = t.shape[:2]
        t = t.reshape(b_, l_, GLA_HEADS, d).transpose(0, 2, 1, 3)
        return t.reshape(b_, GLA_HEADS, l_ // GLA_CHUNK, GLA_CHUNK, d)

    def unchunk(t, flip):
        b_, h_, n_, c_, d_ = t.shape
        t = t.reshape(b_, h_, n_ * c_, d_).transpose(0, 2, 1, 3)
        return flip(t.reshape(b_, n_ * c_, h_ * d_))

    def log_decay(gd, dr):
        z = gd[..., dr * GLA_GATE_RANK:(dr + 1) * GLA_GATE_RANK] @ gk_up[dr] + gk_b[dr]
        return jax.nn.log_sigmoid(z.astype(f32)) / GLA_GATE_NORM

    q = q * GLA_DK ** -0.5
    qc = qc * GLA_DK ** -0.5
    s0 = jnp.zeros((kc.shape[0], GLA_HEADS, GLA_DK, GLA_DV), f32)
    o, oc = 0.0, 0.0
    for dr in range(2):
        flip = _flip if dr else _ident
        strict = dr == 1
        kcn, vcn = chunks(kc, GLA_DK, flip), chunks(vc, GLA_DV, flip)
        bcn = jnp.cumsum(chunks(log_decay(gkdc, dr), GLA_DK, flip), axis=3)
        starts_c, s_ctx = gla_states(kcn, vcn, bcn, s0)
        kn, vn = chunks(k, GLA_DK, flip), chunks(v, GLA_DV, flip)
        bn = jnp.cumsum(chunks(log_decay(gkd, dr), GLA_DK, flip), axis=3)
        starts, _ = gla_states(kn, vn, bn, s_ctx)
        o = o + unchunk(gla_readout(chunks(q, GLA_DK, flip), kn, vn, bn, starts, strict), flip)
        if ctx_out:
            oc = oc + unchunk(gla_readout(chunks(qc, GLA_DK, flip), kcn, vcn, bcn, starts_c, strict), flip)
    return o, (oc if ctx_out else None)


def gla_output(o, go, norm_w):
    b_, l_ = o.shape[:2]
    oh = rmsnorm(o.reshape(b_, l_, GLA_HEADS, GLA_DV), norm_w).reshape(b_, l_, GLA_VAL_WIDTH)
    return oh.astype(go.dtype) * jax.nn.silu(go)


def token_mixer(h, hc, w_in, lam_re, lam_im, log_dt, b_re, b_im, c_re, c_im, d_skip,
                glu_w, gk_up, gk_b, gla_norm_w, w_s5_proj, w_gla_proj, w_out, ctx_out):
    cuts = [int(v) for v in np.cumsum(IN_SPLITS)[:-1]]
    u, q, k, v, go, gkd, gs, gg = jnp.split(h @ w_in, cuts, axis=-1)
    uc, qc, kc, vc, goc, gkdc, gsc, ggc = jnp.split(hc @ w_in, cuts, axis=-1)
    ys, ysc = s5_bidir(u, uc, lam_re, lam_im, log_dt, b_re, b_im, c_re, c_im, d_skip, ctx_out)
    yg, ygc = gla_bidir(q, k, v, gkd, qc, kc, vc, gkdc, gk_up, gk_b, ctx_out)

    def merge(ys_, yg_, go_, gs_, gg_):
        a = s5_glu(ys_, glu_w) @ w_s5_proj
        b = gla_output(yg_, go_, gla_norm_w) @ w_gla_proj
        return (jax.nn.sigmoid(gs_) * a + jax.nn.sigmoid(gg_) * b) @ w_out

    y = merge(ys, yg, go, gs, gg)
    yc = merge(ysc, ygc, goc, gsc, ggc) if ctx_out else None
    return y, yc


def moe(h, router_w, router_bias, w_gate, w_up, w_down, s_gate, s_up, s_down):
    shp = h.shape
    t = h.reshape(-1, D_MODEL)
    n_tok = t.shape[0]
    scores = jax.nn.sigmoid((t @ router_w).astype(jnp.float32))
    choice = scores + router_bias.astype(jnp.float32)
    grp = choice.reshape(n_tok, N_GROUPS, N_EXPERTS // N_GROUPS)
    grp_score = lax.top_k(grp, 2)[0].sum(-1)
    _, gidx = lax.top_k(grp_score, TOPK_GROUPS)
    gmask = jax.nn.one_hot(gidx, N_GROUPS).sum(axis=1) > 0
    emask = jnp.repeat(gmask, N_EXPERTS // N_GROUPS, axis=1)
    _, eidx = lax.top_k(jnp.where(emask, choice, -jnp.inf), TOP_K)
    wts = jnp.take_along_axis(scores, eidx, axis=1)
    wts = wts / jnp.sum(wts, axis=-1, keepdims=True) * ROUTED_SCALE

    n_asg = n_tok * TOP_K
    n_blocks = (n_asg + N_EXPERTS * (EXPERT_BLOCK - 1) + EXPERT_BLOCK - 1) // EXPERT_BLOCK
    flat_e = eidx.reshape(-1)
    order = jnp.argsort(flat_e)
    sorted_e = flat_e[order]
    sizes = jnp.bincount(flat_e, length=N_EXPERTS)
    padded = (sizes + EXPERT_BLOCK - 1) // EXPERT_BLOCK * EXPERT_BLOCK
    pad_ends = jnp.cumsum(padded)
    rank = jnp.arange(n_asg) - (jnp.cumsum(sizes) - sizes)[sorted_e]
    dest = (pad_ends - padded)[sorted_e] + rank
    row_buf = jnp.full((n_blocks * EXPERT_BLOCK,), n_tok, jnp.int32).at[dest].set((order // TOP_K).astype(jnp.int32))
    w_buf = jnp.zeros((n_blocks * EXPERT_BLOCK,), jnp.float32).at[dest].set(wts.reshape(-1)[order])
    block_e = jnp.minimum(jnp.searchsorted(pad_ends, jnp.arange(n_blocks) * EXPERT_BLOCK, side='right'), N_EXPERTS - 1)
    t_pad = jnp.concatenate([t, jnp.zeros((1, D_MODEL), t.dtype)], axis=0)

    def expert_block(args):
        rows, e = args
        xb = t_pad[rows]
        hid = jax.nn.silu(xb @ w_gate[e]) * (xb @ w_up[e])
        return hid @ w_down[e]

    y_buf = lax.map(expert_block, (row_buf.reshape(n_blocks, EXPERT_BLOCK), block_e))
    y_buf = y_buf.reshape(n_blocks * EXPERT_BLOCK, D_MODEL) * w_buf[:, None].astype(t.dtype)
    routed = jax.ops.segment_sum(y_buf, row_buf, num_segments=n_tok + 1)[:n_tok]
    shared = (jax.nn.silu(t @ s_gate) * (t @ s_up)) @ s_down
    return (routed + shared).reshape(shp)


def setup_inputs(seed: int = 0) -> dict:
    key = jax.random.key(seed)
    ks = iter(list(jax.random.split(key, 40)))
    f32 = jnp.float32

    def nrm(shape, scale):
        return jax.random.normal(next(ks), shape, f32) * scale

    L, D, E, F = DEPTH, D_MODEL, N_EXPERTS, D_EXPERT
    G, P, H = S5_GROUPS, S5_STATE, S5_GROUP
    lam_im_init = jnp.pi * jnp.arange(P, dtype=f32)
    return {
        'x': nrm((BATCH, SEQ, D), 1.0),
        'c': nrm((BATCH, D), 1.0),
        'ctx': nrm((BATCH, CTX_LEN, D), 1.0),
        'c_ctx': nrm((D,), 1.0),
        'ada_w': nrm((L, D, N_MOD * D), 0.5 * D ** -0.5),
        'ada_b': nrm((L, N_MOD * D), 0.01),
        'norm1_w': 1.0 + nrm((L, D), 0.02),
        'norm2_w': 1.0 + nrm((L, D), 0.02),
        'w_in': nrm((L, D, IN_WIDTH), D ** -0.5),
        's5_lam_re': -0.5 + nrm((L, 2, G, P), 0.01),
        's5_lam_im': lam_im_init + nrm((L, 2, G, P), 0.01),
        's5_log_dt': jax.random.uniform(next(ks), (L, 2, G), f32, math.log(S5_DT_MIN), math.log(S5_DT_MAX)),
        's5_b_re': nrm((L, 2, G, P, H), (2 * H) ** -0.5),
        's5_b_im': nrm((L, 2, G, P, H), (2 * H) ** -0.5),
        's5_c_re': nrm((L, 2, G, H, P), P ** -0.5),
        's5_c_im': nrm((L, 2, G, H, P), P ** -0.5),
        's5_d': nrm((L, S5_WIDTH), 1.0),
        's5_glu_w': nrm((L, S5_WIDTH, S5_WIDTH), S5_WIDTH ** -0.5),
        'gla_gk_up': nrm((L, 2, GLA_GATE_RANK, GLA_KEY_WIDTH), GLA_GATE_RANK ** -0.5),
        'gla_gk_b': nrm((L, 2, GLA_KEY_WIDTH), 0.1),
        'gla_norm_w': 1.0 + nrm((L, GLA_DV), 0.02),
        'w_s5_proj': nrm((L, S5_WIDTH, D), S5_WIDTH ** -0.5),
        'w_gla_proj': nrm((L, GLA_VAL_WIDTH, D), GLA_VAL_WIDTH ** -0.5),
        'w_out': nrm((L, D, D), D ** -0.5),
        'router_w': nrm((L, D, E), D ** -0.5),
        'router_bias': nrm((L, E), 0.01),
        'exp_w_gate': nrm((L, E, D, F), D ** -0.5),
        'exp_w_up': nrm((L, E, D, F), D ** -0.5),
        'exp_w_down': nrm((L, E, F, D), F ** -0.5),
        'sh_w_gate': nrm((L, D, D_SHARED), D ** -0.5),
        'sh_w_up': nrm((L, D, D_SHARED), D ** -0.5),
        'sh_w_down': nrm((L, D_SHARED, D), D_SHARED ** -0.5),
        'final_norm_w': 1.0 + nrm((D,), 0.02),
    }


def reference(x, c, ctx, c_ctx, ada_w, ada_b, norm1_w, norm2_w, w_in, s5_lam_re, s5_lam_im,
              s5_log_dt, s5_b_re, s5_b_im, s5_c_re, s5_c_im, s5_d, s5_glu_w, gla_gk_up, gla_gk_b,
              gla_norm_w, w_s5_proj, w_gla_proj, w_out, router_w, router_bias, exp_w_gate, exp_w_up,
              exp_w_down, sh_w_gate, sh_w_up, sh_w_down, final_norm_w):
    for i in range(DEPTH):
        last = i == DEPTH - 1
        mod = jax.nn.silu(c) @ ada_w[i] + ada_b[i]
        mod_c = jax.nn.silu(c_ctx) @ ada_w[i] + ada_b[i]
        sh1, sc1, g1, sh2, sc2, g2 = jnp.split(mod[:, None, :], N_MOD, axis=-1)
        csh1, csc1, cg1, csh2, csc2, cg2 = jnp.split(mod_c, N_MOD, axis=-1)
        h = modulate(x, norm1_w[i], sh1, sc1)
        hc = modulate(ctx, norm1_w[i], csh1, csc1)
        y, yc = token_mixer(h, hc, w_in[i], s5_lam_re[i], s5_lam_im[i], s5_log_dt[i], s5_b_re[i],
                            s5_b_im[i], s5_c_re[i], s5_c_im[i], s5_d[i], s5_glu_w[i], gla_gk_up[i],
                            gla_gk_b[i], gla_norm_w[i], w_s5_proj[i], w_gla_proj[i], w_out[i],
                            ctx_out=not last)
        x = x + g1 * y
        x = x + g2 * moe(modulate(x, norm2_w[i], sh2, sc2), router_w[i], router_bias[i],
                         exp_w_gate[i], exp_w_up[i], exp_w_down[i], sh_w_gate[i], sh_w_up[i], sh_w_down[i])
        if not last:
            ctx = ctx + cg1 * yc
            ctx = ctx + cg2 * moe(modulate(ctx, norm2_w[i], csh2, csc2), router_w[i], router_bias[i],
                                  exp_w_gate[i], exp_w_up[i], exp_w_down[i], sh_w_gate[i], sh_w_up[i], sh_w_down[i])
    return rmsnorm(x, final_norm_w)
```

```python
import contextlib
import math
import numpy as np
import concourse.bass as bass
import concourse.mybir as mybir
from concourse.bass_utils import run_bass_kernel_spmd

F32 = mybir.dt.float32
F32R = mybir.dt.float32r
BF16 = mybir.dt.bfloat16
AF = mybir.ActivationFunctionType
ALU = mybir.AluOpType
AX = mybir.AxisListType

D = 2048
KT = 16
NOWN = 1024
NCTX = 256
EPS = 1e-6
NRING = 24


class Tok:
    __slots__ = ("w", "r")

    def __init__(self):
        self.w = []
        self.r = {}


class FW:
    def __init__(self, nc, es):
        self.nc = nc
        self.es = es
        self.streams = {e: [] for e in ("pe", "act", "dve", "pool", "sp")}
        self.sem = {e: es.enter_context(nc.semaphore("s_" + e)) for e in ("pe", "act", "dve", "pool")}
        self.cnt = {e: 0 for e in self.sem}
        self.waited = {e: {} for e in self.streams}
        self.dring = [es.enter_context(nc.semaphore(f"dq{i}")) for i in range(NRING)]
        self.dcnt = [0] * NRING
        self.dnext = 0
        self.nsb = 0

    def sb(self, shape, dtype=F32, name=None):
        self.nsb += 1
        return self.es.enter_context(self.nc.sbuf_tensor(f"{name or 'sb'}_{self.nsb}", list(shape), dtype))

    def ps(self, shape, dtype=F32, name=None):
        self.nsb += 1
        return self.es.enter_context(self.nc.psum_tensor(name or f"ps{self.nsb}", list(shape), dtype))

    def dram(self, name, shape, dtype=F32, kind="Internal"):
        return self.nc.dram_tensor(name, list(shape), dtype, kind=kind).ap()

    def _wait(self, eng, deps):
        for (sem, val, src) in deps:
            if src is not None and src == eng and eng == "pe":
                continue
            key = id(sem)
            if self.waited[eng].get(key, 0) >= val:
                continue
            self.waited[eng][key] = val
            self.streams[eng].append(lambda e, sem=sem, val=val: e.wait_ge(sem, val))

    @staticmethod
    def _deps(reads, writes, acc=False):
        d = []
        for t in reads:
            d.extend(t.w)
        for t in writes:
            if not acc:
                d.extend(t.w)
            d.extend(t.r.values())
        return d

    @staticmethod
    def _mark(tok, reads, writes, key, acc=False):
        for t in reads:
            t.r[key] = tok
        for t in writes:
            if acc:
                t.w.append(tok)
            else:
                t.w = [tok]
            t.r = {}

    def op(self, eng, fn, reads=(), writes=(), acc=False):
        self._wait(eng, self._deps(reads, writes, acc))
        self.cnt[eng] += 1
        sem = self.sem[eng]
        self.streams[eng].append(lambda e, fn=fn, sem=sem: fn(e).then_inc(sem, 1))
        self._mark((sem, self.cnt[eng], eng), reads, writes, eng, acc)

    def dma(self, q, out, in_, reads=(), writes=(), acc=False, **kw):
        i = self.dnext
        self.dnext = (i + 1) % NRING
        sem = self.dring[i]
        deps = self._deps(reads, writes, acc)
        if self.dcnt[i]:
            deps.append((sem, self.dcnt[i], None))
        self._wait(q, deps)
        self.dcnt[i] += 16
        self.streams[q].append(lambda e, out=out, in_=in_, sem=sem, kw=kw: e.dma_start(out=out, in_=in_, **kw).then_inc(sem, 16))
        self._mark((sem, self.dcnt[i], None), reads, writes, ("d", i), acc)

    def idma(self, out, out_off, in_, in_off, reads=(), writes=(), acc=False):
        i = self.dnext
        self.dnext = (i + 1) % NRING
        sem = self.dring[i]
        deps = self._deps(reads, writes, acc)
        if self.dcnt[i]:
            deps.append((sem, self.dcnt[i], None))
        self._wait("pool", deps)
        self.dcnt[i] += 16
        self.streams["pool"].append(lambda e, out=out, in_=in_, sem=sem: e.indirect_dma_start(out, out_off, in_, in_off).then_inc(sem, 16))
        self._mark((sem, self.dcnt[i], None), reads, writes, ("d", i), acc)

    def barrier(self):
        engs = ("pe", "act", "dve", "pool")
        deps = [(self.sem[e], self.cnt[e], e) for e in engs if self.cnt[e]]
        deps += [(self.dring[i], self.dcnt[i], None) for i in range(NRING) if self.dcnt[i]]
        for e in engs + ("sp",):
            self._wait(e, deps)

    @contextlib.contextmanager
    def phase(self):
        old = self.es
        with contextlib.ExitStack() as es2:
            self.es = es2
            try:
                yield
            finally:
                self.barrier()
                self.es = old

    def finish(self, final_toks):
        deps = []
        for t in final_toks:
            deps.extend(t.w)
        self._wait("sp", deps)
        nc = self.nc
        with nc.Block() as block:
            @block.sync
            def _(e):
                for f in self.streams["sp"]:
                    f(e)

            @block.tensor
            def _(e):
                for f in self.streams["pe"]:
                    f(e)

            @block.scalar
            def _(e):
                for f in self.streams["act"]:
                    f(e)

            @block.vector
            def _(e):
                for f in self.streams["dve"]:
                    f(e)

            @block.gpsimd
            def _(e):
                for f in self.streams["pool"]:
                    f(e)


def r32(ap):
    return ap.bitcast(F32R)


def build(debug=()):
    nc = bass.Bass("TRN2", target_bir_lowering=False, dynamic_dma_scratch_size=4096)
    nc.dge_precook = False
    es = contextlib.ExitStack()
    with es:
        fw = FW(nc, es)
        _build(nc, fw, set(debug))
    return nc


def _build(nc, fw, debug):
    def din(name, shape, dtype=F32):
        return nc.dram_tensor(name, list(shape), dtype, kind="ExternalInput").ap()

    def dout(name, shape, dtype=F32):
        return nc.dram_tensor(name, list(shape), dtype, kind="ExternalOutput").ap()

    x_own = din("x_own", [NOWN, D])
    x_oth = din("x_oth", [NOWN, D])
    x_ctx = din("x_ctx", [NCTX, D])
    cT = din("cT", [128, KT, 2])
    ada_w = din("ada_w", [D, 6 * D])
    ada_b = din("ada_b", [1, 6 * D])
    n1w = din("n1w", [128, KT])
    w_in = din("w_in", [D, 8224])
    w_gk = din("w_gk", [D, 128])
    gk_up = din("gk_up", [2, 16, 512])
    gk_b = din("gk_b", [2, 1, 512])
    gla_nw = din("gla_nw", [128, 2])
    s5_l = din("s5_l", [2, 3, 64, 64])
    s5_bc = din("s5_bc", [2, 4, 64, 1024])
    s5_dv = din("s5_dv", [128, 64])
    glu_w = din("glu_w", [1024, 1024])
    w_s5p = din("w_s5p", [1024, D])
    w_glap = din("w_glap", [1024, D])
    w_out = din("w_out", [D, D])
    n2w = din("n2w", [128, KT])
    n2row = din("n2row", [1, D])
    router_w = din("router_w", [D, 64])
    router_b = din("router_b", [1, 64])
    exp_g = din("exp_g", [64, D, 512])
    exp_u = din("exp_u", [64, D, 512])
    exp_d = din("exp_d", [64, 512, D])
    sh_g = din("sh_g", [D, 512])
    sh_u = din("sh_u", [D, 512])
    sh_d = din("sh_d", [512, D])
    fnw = din("fnw", [1, D])

    out = dout("out", [NOWN, D])
    final = []

    ident = fw.sb([128, 128], F32, "ident")
    t_ident = Tok()
    fw.op("pool", lambda e: e.memset(ident[:], 0.0), writes=[t_ident])
    fw.op("pool", lambda e: e.affine_select(out=ident[:], in_=ident[:], pattern=[[-1, 128]], compare_op=ALU.not_equal,
                                            fill=1.0, base=0, channel_multiplier=1), reads=[t_ident], writes=[t_ident])

    psall = fw.ps([128, 8, 512], F32, "psall")
    psb = [psall[:, i, :] for i in range(8)]
    t_ps = [Tok() for _ in range(8)]
    psi = [0]

    def next_ps():
        i = psi[0]
        psi[0] = (i + 1) % 8
        return psb[i], t_ps[i]

    modT = fw.sb([128, 96, 2], F32, "modT")
    t_modT = Tok()
    s1 = fw.sb([128, KT, 2], F32, "s1")
    t_s1 = Tok()
    NWB = 2
    WB = {}
    wbi = [0]

    def alloc_wblk(kt=KT):
        WB["buf"] = [fw.sb([128, kt, 512], F32R, f"wblk{fw.nsb}_{i}") for i in range(NWB)]
        WB["tok"] = [Tok() for _ in range(NWB)]

    def load_w(src, col0, ncols, kt=KT):
        wblk, t_wblk = WB["buf"], WB["tok"]
        i = wbi[0]
        wbi[0] = (i + 1) % NWB
        v = src.rearrange("(k p) n -> p k n", p=128)
        for k in range(kt):
            fw.dma("sp", wblk[i][:, k, 0:ncols], src[k * 128:(k + 1) * 128, col0:col0 + ncols].bitcast(F32R), writes=[t_wblk[i]], acc=(k > 0))
        return wblk[i], t_wblk[i]

    mod_d = fw.dram("mod_d", [2, 6 * D])
    t_modd = Tok()
    with fw.phase():
        alloc_wblk()
        cs = fw.sb([128, KT, 128], F32R, "cs")
        t_cs = Tok()
        cin = fw.sb([128, KT, 2], F32, "cin")
        t_cin = Tok()
        for k in range(KT):
            fw.op("dve", lambda e, k=k: e.tensor_scalar(out=cs[:, k, :], in0=ident[:], scalar1=0.0, scalar2=None, op0=ALU.mult),
                  reads=[t_ident], writes=[t_cs])
        fw.dma("sp", cin[:], cT, writes=[t_cin])
        fw.op("act", lambda e: e.activation(out=cs[:, :, 0:2], in_=cin[:], func=AF.Silu), reads=[t_cin], writes=[t_cs])
        mblk = [fw.sb([2, 512], F32, f"mblk{i}") for i in range(2)]
        t_mblk = [Tok() for _ in range(2)]
        ablk = [fw.sb([2, 512], F32, f"ablk{i}") for i in range(2)]
        t_ablk = [Tok() for _ in range(2)]
        psT, tpsT = next_ps()
        for cb in range(24):
            wb, twb = load_w(ada_w, cb * 512, 512)
            i = cb % 2
            fw.dma("sp", ablk[i][0:1, :], ada_b[:, cb * 512:(cb + 1) * 512], writes=[t_ablk[i]])
            fw.dma("sp", ablk[i][1:2, :], ada_b[:, cb * 512:(cb + 1) * 512], writes=[t_ablk[i]])
            ps, tps = next_ps()
            if ps is psT:
                ps, tps = next_ps()
            for k in range(KT):
                fw.op("pe", lambda e, ps=ps, wb=wb, k=k: e.matmul(ps[:, :], lhsT=cs[:, k, :], rhs=wb[:, k, :],
                                                                   start=(k == 0), stop=(k == KT - 1)),
                      reads=[t_cs, twb], writes=[tps])
            fw.op("dve", lambda e, ps=ps, i=i: e.tensor_tensor(out=mblk[i][:], in0=ps[0:2, :], in1=ablk[i][:], op=ALU.add),
                  reads=[tps, t_ablk[i]], writes=[t_mblk[i]])
            fw.dma("sp", mod_d[:, cb * 512:(cb + 1) * 512], mblk[i][:], reads=[t_mblk[i]], writes=[t_modd])
            for jj in range(4):
                j = cb * 4 + jj
                fw.op("pe", lambda e, i=i, j=j, jj=jj: e.transpose(out=psT[:, 2 * j:2 * j + 2], in_=mblk[i][0:2, jj * 128:(jj + 1) * 128],
                                                                    identity=ident[0:2, 0:2]),
                      reads=[t_mblk[i], t_ident], writes=[tpsT])
        fw.op("dve", lambda e: e.tensor_copy(out=modT[:].rearrange("p j r -> p (j r)"), in_=psT[:, 0:192]),
              reads=[tpsT], writes=[t_modT])
        n1 = fw.sb([128, KT], F32, "n1")
        t_n1 = Tok()
        fw.dma("sp", n1[:], n1w, writes=[t_n1])
        for r in range(2):
            fw.op("dve", lambda e, r=r: e.scalar_tensor_tensor(out=s1[:, :, r], in0=modT[:, KT:2 * KT, r], scalar=1.0, in1=n1[:],
                                                                op0=ALU.add, op1=ALU.mult),
                  reads=[t_modT, t_n1], writes=[t_s1])
        if "mod" in debug:
            dbg_modT = dout("dbg_modT", [128, 192])
            t = Tok()
            fw.dma("sp", dbg_modT, modT[:].rearrange("p j r -> p (j r)"), reads=[t_modT], writes=[t])
            final.append(t)

    with fw.phase():
        alloc_wblk()
        hT = fw.sb([128, KT, 1024], F32R, "hT")
        t_hT = Tok()
        xt = [fw.sb([128, D], F32, f"xt{i}") for i in range(2)]
        t_xt = [Tok() for _ in range(2)]
        junk = fw.sb([128, D], BF16, "junk")
        t_junk = Tok()
        st = fw.sb([128, 4], F32, "st")
        t_st = Tok()
        xti = [0]

        def norm_block(src, ntile, r):
            for ti in range(ntile):
                i = xti[0]
                xti[0] = 1 - i
                fw.dma("sp", xt[i][:], src[ti * 128:(ti + 1) * 128, :], writes=[t_xt[i]])
                fw.op("act", lambda e, i=i: e.activation(out=junk[:], in_=xt[i][:], func=AF.Square, accum_out=st[:, 0:1]),
                      reads=[t_xt[i]], writes=[t_junk, t_st])
                fw.op("act", lambda e: e.activation(out=st[:, 1:2], in_=st[:, 0:1], func=AF.Sqrt, scale=1.0 / D, bias=EPS),
                      reads=[t_st], writes=[t_st])
                fw.op("dve", lambda e: e.reciprocal(out=st[:, 2:3], in_=st[:, 1:2]), reads=[t_st], writes=[t_st])
                fw.op("dve", lambda e, i=i: e.tensor_scalar(out=xt[i][:], in0=xt[i][:], scalar1=st[:, 2:3], scalar2=None, op0=ALU.mult),
                      reads=[t_st, t_xt[i]], writes=[t_xt[i]])
                for kb in range(4):
                    ps, tps = next_ps()
                    for kk in range(4):
                        k = kb * 4 + kk
                        fw.op("pe", lambda e, ps=ps, i=i, k=k, kk=kk: e.transpose(out=ps[:, kk * 128:(kk + 1) * 128],
                                                                                  in_=xt[i][:, k * 128:(k + 1) * 128], identity=ident[:]),
                              reads=[t_xt[i], t_ident], writes=[tps])
                    for kk in range(4):
                        k = kb * 4 + kk
                        fw.op("act", lambda e, ps=ps, k=k, kk=kk, ti=ti: e.activation(out=hT[:, k, ti * 128:(ti + 1) * 128],
                                                                                      in_=ps[:, kk * 128:(kk + 1) * 128], func=AF.Identity,
                                                                                      scale=s1[:, k, r:r + 1], bias=modT[:, k, r:r + 1]),
                              reads=[tps, t_s1, t_modT], writes=[t_hT], acc=(ti + k > 0))

        NB = {"ctx": 32, "own": 128, "oth": 128}
        TOK0 = {"ctx": 0, "own": 256, "oth": 1280}
        u2_d = {b: fw.dram("u2_" + b, [8, 16, 64, NB[b]], BF16) for b in NB}
        qT_d = fw.dram("qT_d", [512, NOWN])
        kT_d = fw.dram("kT_d", [512, NOWN])
        ktok_d = fw.dram("ktok_d", [2304, 512])
        vtok_d = fw.dram("vtok_d", [2304, 1024])
        gkdT_d = fw.dram("gkdT_d", [32, 2304])
        sgoT_d = fw.dram("sgoT_d", [1024, NOWN])
        sgsT_d = fw.dram("sgsT_d", [2048, NOWN])
        sggT_d = fw.dram("sggT_d", [2048, NOWN])
        t_scr = Tok()

        NST = 4
        stg = [fw.sb([128, 512], F32, f"stg{i}") for i in range(NST)]
        t_stg = [Tok() for _ in range(NST)]
        stgb = [fw.sb([128, 8, 64], BF16, f"stgb{i}") for i in range(2)]
        t_stgb = [Tok() for _ in range(2)]
        sti = [0, 0]

        def next_stg():
            i = sti[0]
            sti[0] = (i + 1) % NST
            return stg[i], t_stg[i]

        def fm_linear(W, col0, ncols, ntok, evac):
            wb, twb = load_w(W, col0, ncols)
            for m in range(ncols // 128):
                for c0 in range(0, ntok, 512):
                    cw = min(512, ntok - c0)
                    ps, tps = next_ps()
                    for k in range(KT):
                        fw.op("pe", lambda e, ps=ps, wb=wb, k=k, m=m, c0=c0, cw=cw: e.matmul(
                            ps[:, 0:cw], lhsT=wb[:, k, m * 128:(m + 1) * 128], rhs=hT[:, k, c0:c0 + cw],
                            start=(k == 0), stop=(k == KT - 1)), reads=[twb, t_hT], writes=[tps])
                    evac(ps, tps, m, c0, cw)

        def tm_linear(W, col0, ncols, ntok, evac):
            wb, twb = load_w(W, col0, ncols)
            for ti in range(ntok // 128):
                ps, tps = next_ps()
                for k in range(KT):
                    fw.op("pe", lambda e, ps=ps, wb=wb, k=k, ti=ti: e.matmul(
                        ps[:, 0:ncols], lhsT=hT[:, k, ti * 128:(ti + 1) * 128], rhs=wb[:, k, 0:ncols],
                        start=(k == 0), stop=(k == KT - 1)), reads=[twb, t_hT], writes=[tps])
                evac(ps, tps, ti)

        def evac_to(dst, func=AF.Identity, scale=1.0, rows=128):
            def mk(ch0):
                def ev(ps, tps, m, c0, cw):
                    sg, tsg = next_stg()
                    fw.op("act", lambda e: e.activation(out=sg[0:rows, 0:cw], in_=ps[0:rows, 0:cw], func=func, scale=scale),
                          reads=[tps], writes=[tsg])
                    r0 = ch0 + m * 128
                    fw.dma("sp", dst[r0:r0 + rows, c0:c0 + cw], sg[0:rows, 0:cw], reads=[tsg], writes=[t_scr], acc=True)
                return ev
            return mk

        def evac_u(blk):
            def mk(g0):
                def ev(ps, tps, m, c0, cw):
                    i = sti[1]
                    sti[1] = 1 - i
                    nn = cw // 8
                    fw.op("act", lambda e: e.activation(out=stgb[i][:, :, 0:nn],
                                                        in_=ps[:, 0:cw].rearrange("p (n s) -> p s n", s=8), func=AF.Identity),
                          reads=[tps], writes=[t_stgb[i]])
                    for gl in range(8):
                        g = g0 + m * 8 + gl
                        fw.dma("sp", u2_d[blk][:, :, g, c0 // 8:c0 // 8 + nn].rearrange("s h n -> h s n"),
                               stgb[i][gl * 16:(gl + 1) * 16, :, 0:nn], reads=[t_stgb[i]], writes=[t_scr], acc=True)
                return ev
            return mk

        def evac_tok(dst, tok0, c0):
            def ev(ps, tps, ti):
                sg, tsg = next_stg()
                fw.op("dve", lambda e: e.tensor_copy(out=sg[:, :], in_=ps[:, :]), reads=[tps], writes=[tsg])
                fw.dma("sp", dst[tok0 + ti * 128:tok0 + (ti + 1) * 128, c0:c0 + 512], sg[:, :], reads=[tsg], writes=[t_scr], acc=True)
            return ev

        def project(blk, src, ntile, r, full):
            ntok = ntile * 128
            norm_block(src, ntile, r)
            tok0 = TOK0[blk]
            for cb in range(2):
                fm_linear(w_in, cb * 512, 512, ntok, evac_u(blk)(cb * 32))
            for cb in range(2):
                tm_linear(w_in, 2048 + cb * 512, 512, ntok, evac_tok(vtok_d, tok0, cb * 512))
            tm_linear(w_in, 1536, 512, ntok, evac_tok(ktok_d, tok0, 0))
            fm_linear(w_gk, 0, 128, ntok, evac_to(gkdT_d[:, tok0:tok0 + ntok], rows=32)(0))
            if full:
                fm_linear(w_in, 1024, 512, ntok, evac_to(qT_d, scale=128.0 ** -0.5)(0))
                fm_linear(w_in, 1536, 512, ntok, evac_to(kT_d)(0))
                for cb in range(2):
                    fm_linear(w_in, 3072 + cb * 512, 512, ntok, evac_to(sgoT_d, func=AF.Silu)(cb * 512))
                for cb in range(4):
                    fm_linear(w_in, 4128 + cb * 512, 512, ntok, evac_to(sgsT_d, func=AF.Sigmoid)(cb * 512))
                for cb in range(4):
                    fm_linear(w_in, 6176 + cb * 512, 512, ntok, evac_to(sggT_d, func=AF.Sigmoid)(cb * 512))

        project("own", x_own, 8, 0, True)
        project("oth", x_oth, 8, 0, False)
        project("ctx", x_ctx, 2, 1, False)

        if "proj" in debug:
            for nm, src in (("qT", qT_d), ("kT", kT_d), ("ktok", ktok_d), ("vtok", vtok_d), ("gkdT", gkdT_d),
                            ("sgoT", sgoT_d), ("sgsT", sgsT_d), ("sggT", sggT_d), ("u2own", u2_d["own"]), ("u2ctx", u2_d["ctx"])):
                dd = dout("dbg_" + nm, list(src.shape), src.dtype)
                t = Tok()
                fw.dma("sp", dd, src, reads=[t_scr], writes=[t])
                final.append(t)

    ogT_d = fw.dram("ogT_d", [1024, NOWN])
    t_og = Tok()
    with fw.phase():
        def cmat(name):
            return fw.sb([128, 128], F32, name), Tok()
        BD, t_BD = cmat("BD")
        inclA, t_inclA = cmat("inclA")
        remA, t_remA = cmat("remA")
        inclB, t_inclB = cmat("inclB")
        remB, t_remB = cmat("remB")
        maskA, t_maskA = cmat("maskA")
        maskB, t_maskB = cmat("maskB")
        ones, t_ones = cmat("ones")
        fw.op("pool", lambda e: e.memset(ones[:], 1.0), writes=[t_ones])
        fw.op("pool", lambda e: e.memset(BD[:], 0.0), writes=[t_BD])
        fw.op("pool", lambda e: e.memset(BD[0:64, 0:64], -1.0 / 16), reads=[t_BD], writes=[t_BD])
        fw.op("pool", lambda e: e.memset(BD[64:128, 64:128], -1.0 / 16), reads=[t_BD], writes=[t_BD])
        fw.op("pool", lambda e: e.affine_select(out=inclA[:], in_=BD[:], pattern=[[1, 128]], compare_op=ALU.is_ge, fill=0.0,
                                                base=0, channel_multiplier=-1), reads=[t_BD], writes=[t_inclA])
        fw.op("pool", lambda e: e.affine_select(out=inclB[:], in_=BD[:], pattern=[[-1, 128]], compare_op=ALU.is_ge, fill=0.0,
                                                base=0, channel_multiplier=1), reads=[t_BD], writes=[t_inclB])
        fw.op("pool", lambda e: e.tensor_tensor(out=remA[:], in0=BD[:], in1=inclA[:], op=ALU.subtract), reads=[t_BD, t_inclA], writes=[t_remA])
        fw.op("pool", lambda e: e.tensor_tensor(out=remB[:], in0=BD[:], in1=inclB[:], op=ALU.subtract), reads=[t_BD, t_inclB], writes=[t_remB])
        fw.op("pool", lambda e: e.tensor_scalar(out=maskA[:], in0=inclA[:], scalar1=-16.0, scalar2=None, op0=ALU.mult), reads=[t_inclA], writes=[t_maskA])
        fw.op("pool", lambda e: e.tensor_scalar(out=maskB[:], in0=remA[:], scalar1=-16.0, scalar2=None, op0=ALU.mult), reads=[t_remA], writes=[t_maskB])
        INCL = [(inclA, t_inclA), (inclB, t_inclB)]
        REM = [(remA, t_remA), (remB, t_remB)]
        MASK = [(maskA, t_maskA), (maskB, t_maskB)]

        gksb = [fw.sb([16, 2304], F32, f"gksb{i}") for i in range(2)]
        t_gksb = [Tok(), Tok()]
        gup = [fw.sb([16, 512], F32, f"gup{i}") for i in range(2)]
        gb = [fw.sb([1, 512], F32, f"gb{i}") for i in range(2)]
        t_gp = Tok()
        for dr in range(2):
            fw.dma("sp", gksb[dr][:], gkdT_d[dr * 16:(dr + 1) * 16, :], writes=[t_gksb[dr]])
            fw.dma("sp", gup[dr][:], gk_up[dr], writes=[t_gp], acc=True)
            fw.dma("sp", gb[dr][:], gk_b[dr], writes=[t_gp], acc=True)
        gnw = fw.sb([128, 2], F32, "gnw")
        fw.dma("sp", gnw[:], gla_nw, writes=[t_gp], acc=True)

        ygT = fw.sb([128, 8, NOWN], F32, "ygT")
        t_yg = Tok()
        Sst = fw.sb([128, 4, 256], F32, "Sst")
        t_S = [Tok() for _ in range(4)]
        NLB = 2
        ktok = [fw.sb([128, 512], F32, f"ktok{i}") for i in range(NLB)]
        vtok = [fw.sb([128, 1024], F32, f"vtok{i}") for i in range(NLB)]
        qt = [fw.sb([128, 4, 128], F32, f"qt{i}") for i in range(NLB)]
        kt = [fw.sb([128, 4, 128], F32, f"kt{i}") for i in range(NLB)]
        t_ld = [Tok() for _ in range(NLB)]
        e1 = fw.sb([128, 512], F32, "e1"); t_e1 = Tok()
        Lt = fw.sb([128, 512], F32, "Lt"); t_L = Tok()
        er = fw.sb([128, 512], F32, "er"); t_er = Tok()
        kd = fw.sb([128, 512], F32, "kd"); t_kd = Tok()
        NHB = 2
        eb = [fw.sb([128, 128], F32, f"eb{i}") for i in range(NHB)]; t_eb = [Tok() for _ in range(NHB)]
        enb = [fw.sb([128, 128], F32, f"enb{i}") for i in range(NHB)]; t_enb = [Tok() for _ in range(NHB)]
        qb = [fw.sb([128, 128], F32, f"qb{i}") for i in range(NHB)]; t_qb = [Tok() for _ in range(NHB)]
        kb = [fw.sb([128, 128], F32, f"kb{i}") for i in range(NHB)]; t_kb = [Tok() for _ in range(NHB)]
        sm = [fw.sb([128, 128], F32, f"sm{i}") for i in range(NHB)]; t_sm = [Tok() for _ in range(NHB)]
        hbi = [0]
        lbi = [0]

        for h in range(4):
            fw.op("dve", lambda e, h=h: e.memset(Sst[:, h, :], 0.0), writes=[t_S[h]])

        def gla_tile(dr, row0, own_tok0, first_dir):
            own = own_tok0 is not None
            lb = lbi[0]
            lbi[0] = (lb + 1) % NLB
            fw.dma("sp", ktok[lb][:], ktok_d[row0:row0 + 128, :], writes=[t_ld[lb]])
            fw.dma("sp", vtok[lb][:], vtok_d[row0:row0 + 128, :], writes=[t_ld[lb]], acc=True)
            if own:
                fw.dma("sp", qt[lb][:], qT_d[:, own_tok0:own_tok0 + 128].rearrange("(h p) n -> p h n", p=128), writes=[t_ld[lb]], acc=True)
                fw.dma("sp", kt[lb][:], kT_d[:, own_tok0:own_tok0 + 128].rearrange("(h p) n -> p h n", p=128), writes=[t_ld[lb]], acc=True)
            tld = t_ld[lb]
            incl, t_incl = INCL[dr]
            rem, t_rem = REM[dr]
            msk, t_msk = MASK[dr]
            zps, tz = next_ps()
            fw.op("pe", lambda e: e.matmul(zps[:, :], lhsT=gksb[dr][:, row0:row0 + 128], rhs=gup[dr][:], start=True, stop=False),
                  reads=[t_gksb[dr], t_gp], writes=[tz])
            fw.op("pe", lambda e: e.matmul(zps[:, :], lhsT=ones[0:1, :], rhs=gb[dr][:], start=False, stop=True),
                  reads=[t_ones, t_gp], writes=[tz])
            fw.op("act", lambda e: e.activation(out=e1[:], in_=zps[:, :], func=AF.Exp, scale=-1.0), reads=[tz], writes=[t_e1])
            fw.op("act", lambda e: e.activation(out=Lt[:], in_=e1[:], func=AF.Ln, bias=1.0), reads=[t_e1], writes=[t_L])
            rps, tr = next_ps()
            fw.op("pe", lambda e: e.matmul(rps[:, :], lhsT=rem[:], rhs=Lt[:], start=True, stop=True), reads=[t_rem, t_L], writes=[tr])
            fw.op("act", lambda e: e.activation(out=er[:], in_=rps[:, :], func=AF.Exp), reads=[tr], writes=[t_er])
            fw.op("pool", lambda e: e.tensor_tensor(out=kd[:], in0=ktok[lb][:], in1=er[:], op=ALU.mult), reads=[tld, t_er], writes=[t_kd])
            corder = (0, 1) if dr == 0 else (1, 0)
            def head(h):
                hb = hbi[0]
                hbi[0] = (hb + 1) % NHB
                bps, tb = next_ps()
                fw.op("pe", lambda e: e.matmul(bps[:, 0:128], lhsT=Lt[:, h * 128:(h + 1) * 128], rhs=incl[:], start=True, stop=True),
                      reads=[t_L, t_incl], writes=[tb])
                fw.op("act", lambda e: e.activation(out=eb[hb][:], in_=bps[:, 0:128], func=AF.Exp), reads=[tb], writes=[t_eb[hb]])
                if own:
                    fw.op("act", lambda e: e.activation(out=enb[hb][:], in_=bps[:, 0:128], func=AF.Exp, scale=-1.0), reads=[tb], writes=[t_enb[hb]])
                    fw.op("pool", lambda e: e.tensor_tensor(out=qb[hb][:], in0=qt[lb][:, h, :], in1=eb[hb][:], op=ALU.mult),
                          reads=[tld, t_eb[hb]], writes=[t_qb[hb]])
                    fw.op("pool", lambda e: e.tensor_tensor(out=kb[hb][:], in0=kt[lb][:, h, :], in1=enb[hb][:], op=ALU.mult),
                          reads=[tld, t_enb[hb]], writes=[t_kb[hb]])
                    sps, tsc = next_ps()
                    fw.op("pe", lambda e: e.matmul(sps[:, 0:128], lhsT=kb[hb][:], rhs=qb[hb][:], start=True, stop=True),
                          reads=[t_kb[hb], t_qb[hb]], writes=[tsc])
                    fw.op("dve", lambda e: e.tensor_tensor(out=sm[hb][:], in0=sps[:, 0:128], in1=msk[:], op=ALU.mult),
                          reads=[tsc, t_msk], writes=[t_sm[hb]])
                def chunk(c):
                    cs_ = slice(c * 64, (c + 1) * 64)
                    if own:
                        ops_, to = next_ps()
                        for vt in range(2):
                            vcol = slice(h * 256 + vt * 128, h * 256 + (vt + 1) * 128)
                            oc = slice(vt * 64, (vt + 1) * 64)
                            fw.op("pe", lambda e, vcol=vcol, oc=oc: e.matmul(ops_[:, oc], lhsT=vtok[lb][cs_, vcol], rhs=sm[hb][cs_, cs_],
                                                                              start=True, stop=False), reads=[tld, t_sm[hb]], writes=[to])
                            fw.op("pe", lambda e, vt=vt, oc=oc: e.matmul(ops_[:, oc], lhsT=Sst[:, h, vt * 128:(vt + 1) * 128], rhs=qb[hb][:, cs_],
                                                                          start=False, stop=True), reads=[t_S[h], t_qb[hb]], writes=[to])
                        tk = slice(own_tok0 + c * 64, own_tok0 + (c + 1) * 64)
                        for vt in range(2):
                            oc = slice(vt * 64, (vt + 1) * 64)
                            if first_dir:
                                fw.op("act", lambda e, vt=vt, oc=oc: e.activation(out=ygT[:, h * 2 + vt, tk], in_=ops_[:, oc], func=AF.Identity),
                                      reads=[to], writes=[t_yg], acc=True)
                            else:
                                fw.op("dve", lambda e, vt=vt, oc=oc: e.tensor_tensor(out=ygT[:, h * 2 + vt, tk], in0=ygT[:, h * 2 + vt, tk],
                                                                                      in1=ops_[:, oc], op=ALU.add),
                                      reads=[to], writes=[t_yg], acc=True)
                    dps, td = next_ps()
                    fw.op("pe", lambda e: e.matmul(dps[:, 0:256], lhsT=kd[cs_, h * 128:(h + 1) * 128], rhs=vtok[lb][cs_, h * 256:(h + 1) * 256],
                                                   start=True, stop=True), reads=[t_kd, tld], writes=[td])
                    col = c * 64 + 63 if dr == 0 else c * 64
                    fw.op("dve", lambda e, col=col: e.scalar_tensor_tensor(out=Sst[:, h, :], in0=Sst[:, h, :], scalar=eb[hb][:, col:col + 1],
                                                                            in1=dps[:, 0:256], op0=ALU.mult, op1=ALU.add),
                          reads=[td, t_eb[hb], t_S[h]], writes=[t_S[h]])
                for c in corder:
                    chunk(c)
            for h in range(4):
                head(h)

        for t in range(2):
            gla_tile(0, t * 128, None, True)
        for t in range(8):
            gla_tile(0, 256 + t * 128, t * 128, True)
        fw.barrier()
        for h in range(4):
            fw.op("dve", lambda e, h=h: e.memset(Sst[:, h, :], 0.0), reads=[t_S[h]], writes=[t_S[h]])
        for t in (1, 0):
            gla_tile(1, t * 128, None, False)
        for t in range(7, -1, -1):
            gla_tile(1, 1280 + t * 128, None, False)
        for t in range(7, -1, -1):
            gla_tile(1, 256 + t * 128, t * 128, False)
        fw.barrier()

        if "yg" in debug:
            dd = dout("dbg_ygT", [1024, NOWN])
            t = Tok()
            fw.dma("sp", dd.rearrange("(k p) n -> p k n", p=128), ygT[:], writes=[t])
            final.append(t)

        sq = [fw.sb([128, 512], F32, f"sq{i}") for i in range(2)]; t_sq = [Tok(), Tok()]
        rs = fw.sb([128, 512], F32, "rs"); t_rs = Tok()
        sgo = [fw.sb([128, 512], F32, f"sgo{i}") for i in range(2)]; t_sgo = [Tok(), Tok()]
        ogs = [fw.sb([128, 512], F32, f"ogs{i}") for i in range(2)]; t_ogs = [Tok(), Tok()]
        def go_block(h, c0):
            if True:
                ssp, tss = next_ps()
                for vt in range(2):
                    fw.op("act", lambda e, vt=vt: e.activation(out=sq[vt][:], in_=ygT[:, h * 2 + vt, c0:c0 + 512], func=AF.Square),
                          writes=[t_sq[vt]])
                    fw.op("pe", lambda e, vt=vt: e.matmul(ssp[:, :], lhsT=ones[:], rhs=sq[vt][:], start=(vt == 0), stop=(vt == 1)),
                          reads=[t_ones, t_sq[vt]], writes=[tss])
                fw.op("act", lambda e: e.activation(out=rs[:], in_=ssp[:, :], func=AF.Sqrt, scale=1.0 / 256, bias=EPS), reads=[tss], writes=[t_rs])
                fw.op("dve", lambda e: e.reciprocal(out=rs[:], in_=rs[:]), reads=[t_rs], writes=[t_rs])
                for vt in range(2):
                    r0 = (h * 2 + vt) * 128
                    fw.dma("sp", sgo[vt][:], sgoT_d[r0:r0 + 128, c0:c0 + 512], writes=[t_sgo[vt]])
                    fw.op("dve", lambda e, vt=vt: e.tensor_tensor(out=ogs[vt][:], in0=ygT[:, h * 2 + vt, c0:c0 + 512], in1=rs[:], op=ALU.mult),
                          reads=[t_rs], writes=[t_ogs[vt]])
                    fw.op("dve", lambda e, vt=vt: e.scalar_tensor_tensor(out=ogs[vt][:], in0=ogs[vt][:], scalar=gnw[:, vt:vt + 1], in1=sgo[vt][:],
                                                                          op0=ALU.mult, op1=ALU.mult),
                          reads=[t_sgo[vt], t_gp, t_ogs[vt]], writes=[t_ogs[vt]])
                    fw.dma("sp", ogT_d[r0:r0 + 128, c0:c0 + 512], ogs[vt][:], reads=[t_ogs[vt]], writes=[t_og], acc=True)
        for h in range(4):
            for c0 in (0, 512):
                go_block(h, c0)
        if "og" in debug:
            dd = dout("dbg_ogT", [1024, NOWN])
            t = Tok()
            fw.dma("sp", dd, ogT_d, reads=[t_og], writes=[t])
            final.append(t)

    y2_d = fw.dram("y2_d", [8, 16, 64, 128])
    t_y2 = Tok()
    with fw.phase():
        import math
        U2own = fw.sb([128, 64, 128], BF16, "U2own")
        U2ctx = fw.sb([128, 64, 32], BF16, "U2ctx")
        t_U2 = Tok()
        for (dst, blk) in ((U2own, "own"), (U2ctx, "ctx")):
            fw.dma("sp", dst[:], u2_d[blk].rearrange("s h g n -> (s h) g n"), writes=[t_U2], acc=True)
        Y2 = fw.sb([128, 64, 128], F32, "Y2")
        t_Y2 = Tok()
        dv = fw.sb([128, 64], F32, "dv")
        t_dv = Tok()
        fw.dma("sp", dv[:], s5_dv, writes=[t_dv])
        fw.op("dve", lambda e: e.tensor_tensor(out=Y2[:], in0=U2own[:], in1=dv[:].unsqueeze(2).to_broadcast([128, 64, 128]), op=ALU.mult),
              reads=[t_U2, t_dv], writes=[t_Y2])
        mk = [fw.sb([128, 128], F32, f"mk{i}") for i in range(2)]
        t_mk = Tok()
        onesm = fw.sb([128, 128], F32, "onesm")
        fw.op("pool", lambda e: e.memset(onesm[:], 1.0), writes=[t_mk])
        fw.op("pool", lambda e: e.affine_select(out=mk[0][:], in_=onesm[:], pattern=[[16, 8], [0, 16]], compare_op=ALU.is_ge, fill=0.0,
                                                base=15, channel_multiplier=-1), reads=[t_mk], writes=[t_mk])
        fw.op("pool", lambda e: e.affine_select(out=mk[1][:], in_=onesm[:], pattern=[[-16, 8], [0, 16]], compare_op=ALU.is_ge, fill=0.0,
                                                base=0, channel_multiplier=1), reads=[t_mk], writes=[t_mk])
        Hown = fw.sb([64, 2, 64, 128], BF16, "Hown")
        t_Hown = Tok()
        GB = 8

        def tt(out, a, b, op, reads=(), writes=()):
            fw.op("dve", lambda e: e.tensor_tensor(out=out, in0=a, in1=b, op=op), reads=list(reads), writes=list(writes))

        def dve(fn, reads, writes):
            fw.op("dve", fn, reads=list(reads), writes=list(writes))

        def s5_dir(dr):
          with fw.phase():
            prm = fw.sb([64, 3, 64], F32, "prm")
            bc = fw.sb([64, 4, 1024], F32, "bcp")
            t_prm = Tok()
            sc = fw.sb([64, 16, 64], F32, "sc")
            t_sc = Tok()
            PW = fw.sb([64, 9, 2, 64], F32, "PW")
            t_PW = Tok()
            Bb = fw.sb([64, 2, 1024], F32, "Bb")
            t_Bb = Tok()
            LL = fw.sb([64, 4, 64], F32, "LL")
            t_LL = Tok()

            def S(i):
                return sc[:, i, :]
            fw.dma("sp", prm[:], s5_l[dr].rearrange("t p g -> p t g"), writes=[t_prm])
            fw.dma("sp", bc[:], s5_bc[dr].rearrange("t p f -> p t f"), writes=[t_prm], acc=True)
            R = [t_prm, t_sc]
            Wt = [t_sc]
            lre, lim, ldt = prm[:, 0, :], prm[:, 1, :], prm[:, 2, :]
            fw.op("act", lambda e: e.activation(out=S(0), in_=ldt, func=AF.Exp), reads=R, writes=Wt)
            dve(lambda e: e.scalar_tensor_tensor(out=S(1), in0=lre, scalar=1.0 / 16, in1=S(0), op0=ALU.mult, op1=ALU.mult), R, Wt)
            dve(lambda e: e.scalar_tensor_tensor(out=S(2), in0=lim, scalar=1.0 / 16, in1=S(0), op0=ALU.mult, op1=ALU.mult), R, Wt)
            fw.op("act", lambda e: e.activation(out=S(3), in_=S(1), func=AF.Exp), reads=R, writes=Wt)
            fw.op("act", lambda e: e.activation(out=S(4), in_=S(2), func=AF.Sin), reads=R, writes=Wt)
            fw.op("act", lambda e: e.activation(out=S(5), in_=S(2), func=AF.Sin, scale=-1.0, bias=math.pi / 2), reads=R, writes=Wt)
            tt(S(6), S(3), S(5), ALU.mult, R, Wt)
            tt(S(7), S(3), S(4), ALU.mult, R, Wt)
            for _ in range(4):
                tt(S(8), S(6), S(6), ALU.mult, R, Wt)
                tt(S(9), S(7), S(7), ALU.mult, R, Wt)
                dve(lambda e: e.scalar_tensor_tensor(out=S(7), in0=S(6), scalar=2.0, in1=S(7), op0=ALU.mult, op1=ALU.mult), R, Wt)
                tt(S(6), S(8), S(9), ALU.subtract, R, Wt)
            PWt = [t_PW]
            fw.op("dve", lambda e: e.memset(PW[:, 0, 0, :], 1.0), reads=[t_PW], writes=PWt)
            fw.op("dve", lambda e: e.memset(PW[:, 0, 1, :], 0.0), reads=[t_PW], writes=PWt)
            fw.op("dve", lambda e: e.tensor_copy(out=PW[:, 1, 0, :], in_=S(6)), reads=R + [t_PW], writes=PWt)
            fw.op("dve", lambda e: e.tensor_copy(out=PW[:, 1, 1, :], in_=S(7)), reads=R + [t_PW], writes=PWt)
            RR = R + [t_PW]
            for k in range(2, 9):
                tt(S(8), PW[:, k - 1, 0, :], S(6), ALU.mult, RR, Wt)
                tt(S(9), PW[:, k - 1, 1, :], S(7), ALU.mult, RR, Wt)
                tt(PW[:, k, 0, :], S(8), S(9), ALU.subtract, RR, PWt)
                tt(S(8), PW[:, k - 1, 0, :], S(7), ALU.mult, RR, Wt)
                tt(S(9), PW[:, k - 1, 1, :], S(6), ALU.mult, RR, Wt)
                tt(PW[:, k, 1, :], S(8), S(9), ALU.add, RR, PWt)
            tt(S(8), lre, lre, ALU.mult, RR, Wt)
            tt(S(9), lim, lim, ALU.mult, RR, Wt)
            tt(S(8), S(8), S(9), ALU.add, RR, Wt)
            dve(lambda e: e.reciprocal(out=S(10), in_=S(8)), RR, Wt)
            dve(lambda e: e.tensor_scalar(out=S(11), in0=S(6), scalar1=-1.0, scalar2=None, op0=ALU.add), RR, Wt)
            tt(S(8), S(11), lre, ALU.mult, RR, Wt)
            tt(S(9), S(7), lim, ALU.mult, RR, Wt)
            tt(S(8), S(8), S(9), ALU.add, RR, Wt)
            tt(S(12), S(8), S(10), ALU.mult, RR, Wt)
            tt(S(8), S(7), lre, ALU.mult, RR, Wt)
            tt(S(9), S(11), lim, ALU.mult, RR, Wt)
            tt(S(8), S(8), S(9), ALU.subtract, RR, Wt)
            tt(S(13), S(8), S(10), ALU.mult, RR, Wt)
            tt(S(8), PW[:, 7, 0, :], PW[:, 7, 0, :], ALU.mult, RR, Wt)
            tt(S(9), PW[:, 7, 1, :], PW[:, 7, 1, :], ALU.mult, RR, Wt)
            tt(S(8), S(8), S(9), ALU.add, RR, Wt)
            dve(lambda e: e.reciprocal(out=S(10), in_=S(8)), RR, Wt)
            tt(S(14), PW[:, 7, 0, :], S(10), ALU.mult, RR, Wt)
            dve(lambda e: e.scalar_tensor_tensor(out=S(15), in0=PW[:, 7, 1, :], scalar=-1.0, in1=S(10), op0=ALU.mult, op1=ALU.mult), RR, Wt)
            for q in range(2):
                fw.op("dve", lambda e, q=q: e.tensor_copy(out=LL[:, q, :], in_=PW[:, 8, 0, :]), reads=RR + [t_LL], writes=[t_LL])
            fw.op("dve", lambda e: e.tensor_scalar(out=LL[:, 2, :], in0=PW[:, 8, 1, :], scalar1=-1.0, scalar2=None, op0=ALU.mult), reads=RR + [t_LL], writes=[t_LL])
            fw.op("dve", lambda e: e.tensor_copy(out=LL[:, 3, :], in_=PW[:, 8, 1, :]), reads=RR + [t_LL], writes=[t_LL])

            if "s5p" in debug and dr == 0:
                for nm, src, shp, toks in (("PW", PW[:].rearrange("p k c g -> p (k c g)"), [64, 9 * 2 * 64], [t_PW]),
                                           ("LL", LL[:].rearrange("p k g -> p (k g)"), [64, 256], [t_LL]),
                                           ("sc", sc[:].rearrange("p k g -> p (k g)"), [64, 1024], [t_sc])):
                    dd = dout("dbg_" + nm, shp)
                    t = Tok()
                    fw.dma("sp", dd, src, reads=toks, writes=[t])
                    final.append(t)

            def v3(a):
                return a.rearrange("p (g h) -> p g h", h=16)

            def cmul(tmp, t_tmp, outr, outi, ar, ai, br, bi, G, neg_im=False, RD=(), WR=()):
                t0 = v3(tmp[:, 0, 0:G * 16])
                t1 = v3(tmp[:, 1, 0:G * 16])
                RDD = list(RD) + [t_tmp]
                tt(t0, br, ar, ALU.mult, RDD, [t_tmp])
                tt(t1, bi, ai, ALU.mult, RDD, [t_tmp])
                tt(outr, t0, t1, ALU.subtract, RDD + list(WR), list(WR))
                tt(t0, bi, ar, ALU.mult, RDD, [t_tmp])
                tt(t1, br, ai, ALU.mult, RDD, [t_tmp])
                if neg_im:
                    fw.op("dve", lambda e: e.scalar_tensor_tensor(out=outi, in0=t0, scalar=-1.0, in1=t1, op0=ALU.mult, op1=ALU.subtract),
                          reads=RDD + list(WR), writes=list(WR))
                else:
                    tt(outi, t0, t1, ALU.add, RDD + list(WR), list(WR))

            def bfull(a):
                return a.unsqueeze(2).to_broadcast([64, 64, 16])

            with fw.phase():
                tmpF = fw.sb([64, 2, 1024], F32, "tmpF")
                t_tmpF = Tok()
                cmul(tmpF, t_tmpF, v3(Bb[:, 0, :]), v3(Bb[:, 1, :]), bfull(S(12)), bfull(S(13)), v3(bc[:, 0, :]), v3(bc[:, 1, :]), 64,
                     RD=RR, WR=[t_Bb])
            RB = RR + [t_Bb]

            def pwb(k, c, gs_):
                return PW[:, k, c, gs_].unsqueeze(2).to_broadcast([64, GB, 16])

            def vb(a, gs_):
                return v3(a)[:, gs_, :]

            def gen_Wn(Wn, t_Wn, tmp, t_tmp, gs_):
                for j in range(8):
                    e_ = (7 - j) if dr == 0 else j
                    cmul(tmp, t_tmp, Wn[:, 0, :, j * 16:(j + 1) * 16], Wn[:, 1, :, j * 16:(j + 1) * 16], pwb(e_, 0, gs_), pwb(e_, 1, gs_),
                         vb(Bb[:, 0, :], gs_), vb(Bb[:, 1, :], gs_), GB, RD=RB, WR=[t_Wn])

            with fw.phase():
                Win = fw.sb([128, 64, 2, 64], BF16, "Win")
                t_Win = Tok()
                with fw.phase():
                    Wn = fw.sb([64, 2, GB, 128], F32, "Wn")
                    tmp = fw.sb([64, 2, GB * 16], F32, "tmp")
                    t_Wn, t_tmp = Tok(), Tok()
                    for gb in range(64 // GB):
                        g0 = gb * GB
                        gen_Wn(Wn, t_Wn, tmp, t_tmp, slice(g0, g0 + GB))
                        for g4 in range(GB // 4):
                            def blk4(g4=g4, g0=g0):
                                ps, tps = next_ps()
                                for gi in range(4):
                                    for c in range(2):
                                        fw.op("pe", lambda e, gi=gi, c=c: e.transpose(out=ps[:, (gi * 2 + c) * 64:(gi * 2 + c + 1) * 64],
                                                                                     in_=Wn[:, c, g4 * 4 + gi, :], identity=ident[0:64, 0:64]),
                                              reads=[t_Wn, t_ident], writes=[tps])
                                ga = g0 + g4 * 4
                                fw.op("act", lambda e: e.activation(out=Win[:, ga:ga + 4, :, :].rearrange("p g c q -> p (g c q)"), in_=ps[:, :],
                                                                    func=AF.Identity), reads=[tps], writes=[t_Win], acc=True)
                            blk4()
                if "s5p" in debug and dr == 0:
                    dd = dout("dbg_Win", [128, 64 * 2 * 64], BF16)
                    t = Tok()
                    fw.dma("sp", dd, Win[:].rearrange("p g c q -> p (g c q)"), reads=[t_Win], writes=[t])
                    final.append(t)
                    dd = dout("dbg_Bb", [64, 2048])
                    t = Tok()
                    fw.dma("sp", dd, Bb[:].rearrange("p c f -> p (c f)"), reads=[t_Bb], writes=[t])
                    final.append(t)
                with fw.phase():
                    E = fw.sb([64, 4, 64], F32, "E")
                    Pp = fw.sb([64, 4, 64], F32, "Pp")
                    Tt = fw.sb([64, 2, 64], F32, "Tt")
                    t_E, t_Pp, t_Tt = Tok(), Tok(), Tok()
                    fw.op("dve", lambda e: e.memset(E[:], 0.0), writes=[t_E])
                    if dr == 0:
                        blocks = [(U2ctx, n0, False) for n0 in range(0, 32, 8)] + [(U2own, n0, True) for n0 in range(0, 128, 8)]
                    else:
                        U2oth = fw.sb([128, 64, 128], BF16, "U2oth")
                        fw.dma("sp", U2oth[:], u2_d["oth"].rearrange("s h g n -> (s h) g n"), writes=[t_U2], acc=True)
                        blocks = [(U2ctx, n0, False) for n0 in range(24, -1, -8)] + [(U2oth, n0, False) for n0 in range(120, -1, -8)] + \
                                 [(U2own, n0, True) for n0 in range(120, -1, -8)]
                    pair = [0]

                    def scan_block(U2b, n0, own):
                        pi = pair[0]
                        pair[0] = (pi + 1) % 4
                        tp = [t_ps[2 * pi], t_ps[2 * pi + 1]]
                        Sp = psall[0:64, 2 * pi:2 * pi + 2, :]
                        for g in range(64):
                            for c in range(2):
                                fw.op("pe", lambda e, g=g, c=c: e.matmul(psall[0:64, 2 * pi + c, g * 8:(g + 1) * 8], lhsT=Win[:, g, c, :],
                                                                          rhs=U2b[:, g, n0:n0 + 8], start=True, stop=True),
                                      reads=[t_Win, t_U2], writes=[tp[c]])
                        S4 = Sp.rearrange("p c (g n) -> p c g n", n=8)
                        order = range(8) if dr == 0 else range(7, -1, -1)
                        for j in order:
                            if own:
                                fw.op("dve", lambda e, j=j: e.tensor_copy(out=Hown[:, :, :, n0 + j], in_=E[:, 0:2, :]), reads=[t_E], writes=[t_Hown], acc=True)
                            fw.op("dve", lambda e: e.tensor_tensor(out=Pp[:], in0=E[:], in1=LL[:], op=ALU.mult), reads=[t_E, t_LL, t_Pp], writes=[t_Pp])
                            fw.op("dve", lambda e: e.tensor_tensor(out=Tt[:], in0=Pp[:, 0:2, :], in1=Pp[:, 2:4, :], op=ALU.add), reads=[t_Pp, t_Tt], writes=[t_Tt])
                            fw.op("dve", lambda e, j=j: e.tensor_tensor(out=E[:, 0:2, :], in0=Tt[:], in1=S4[:, :, :, j], op=ALU.add),
                                  reads=[t_Tt, tp[0], tp[1], t_E], writes=[t_E])
                            fw.op("dve", lambda e: e.tensor_copy(out=E[:, 2:4, :], in_=E[:, 1::-1, :]), reads=[t_E], writes=[t_E])

                    for (U2b, n0, own) in blocks:
                        scan_block(U2b, n0, own)

            with fw.phase():
                Rout = fw.sb([64, 64, 2, 128], BF16, "Rout")
                Mm = fw.sb([128, 64, 128], BF16, "Mm")
                C7 = fw.sb([64, 2, 1024], F32, "C7")
                Wn3 = fw.sb([64, 2, GB, 128], F32, "Wn3")
                Rm = fw.sb([64, 2, GB, 128], F32, "Rm")
                tmp = fw.sb([64, 2, GB * 16], F32, "tmp2")
                t_Rout, t_Mm, t_C7, t_Wn, t_Rm, t_tmp = Tok(), Tok(), Tok(), Tok(), Tok(), Tok()
                with fw.phase():
                    tmpF = fw.sb([64, 2, 1024], F32, "tmpF2")
                    t_tmpF = Tok()
                    cmul(tmpF, t_tmpF, v3(C7[:, 0, :]), v3(C7[:, 1, :]), bfull(S(14)), bfull(S(15)), v3(bc[:, 2, :]), v3(bc[:, 3, :]), 64,
                         RD=RR, WR=[t_C7])
                RC = RB + [t_C7]
                for gb in range(64 // GB):
                    g0 = gb * GB
                    gs_ = slice(g0, g0 + GB)
                    gen_Wn(Wn3, t_Wn, tmp, t_tmp, gs_)
                    for j in range(8):
                        q_ = j if dr == 0 else (7 - j)
                        r_ = (j + 1) if dr == 0 else (8 - j)
                        cmul(tmp, t_tmp, Rm[:, 0, :, j * 16:(j + 1) * 16], Rm[:, 1, :, j * 16:(j + 1) * 16], pwb(q_, 0, gs_), pwb(q_, 1, gs_),
                             vb(C7[:, 0, :], gs_), vb(C7[:, 1, :], gs_), GB, neg_im=True, RD=RC, WR=[t_Rm])
                        cmul(tmp, t_tmp, Rout[:, gs_, 0, j * 16:(j + 1) * 16], Rout[:, gs_, 1, j * 16:(j + 1) * 16], pwb(r_, 0, gs_), pwb(r_, 1, gs_),
                             vb(bc[:, 2, :], gs_), vb(bc[:, 3, :], gs_), GB, neg_im=True, RD=RC, WR=[t_Rout])
                    for g4 in range(GB // 4):
                        def blkM(g4=g4, g0=g0):
                            ps2, tps2 = next_ps()
                            for gi in range(4):
                                for c in range(2):
                                    fw.op("pe", lambda e, gi=gi, c=c: e.matmul(ps2[:, gi * 128:(gi + 1) * 128], lhsT=Wn3[:, c, g4 * 4 + gi, :],
                                                                                rhs=Rm[:, c, g4 * 4 + gi, :], start=(c == 0), stop=(c == 1)),
                                          reads=[t_Wn, t_Rm], writes=[tps2])
                            ga = g0 + g4 * 4
                            fw.op("dve", lambda e: e.tensor_tensor(out=Mm[:, ga:ga + 4, :], in0=ps2[:, :].rearrange("p (g f) -> p g f", f=128),
                                                                   in1=mk[dr][:].unsqueeze(1).to_broadcast([128, 4, 128]), op=ALU.mult),
                                  reads=[tps2, t_mk], writes=[t_Mm], acc=True)
                        blkM()
                fw.barrier()
                if "s5p" in debug and dr == 0:
                    for nm, src, shp in (("Rout", Rout[:].rearrange("p g c f -> p (g c f)"), [64, 64 * 2 * 128]),
                                         ("Mm", Mm[:].rearrange("p g f -> p (g f)"), [128, 64 * 128]),
                                         ("Hown", Hown[:].rearrange("p c g n -> p (c g n)"), [64, 2 * 64 * 128])):
                        dd = dout("dbg_" + nm, shp, BF16)
                        t = Tok()
                        fw.dma("sp", dd, src, writes=[t])
                        final.append(t)
                for g4 in range(16):
                    def ro(g4=g4):
                        ps, tps = next_ps()
                        for gi in range(4):
                            g = g4 * 4 + gi
                            oc = slice(gi * 128, (gi + 1) * 128)
                            fw.op("pe", lambda e, g=g, oc=oc: e.matmul(ps[:, oc], lhsT=Rout[:, g, 0, :], rhs=Hown[:, 0, g, :], start=True, stop=False),
                                  reads=[t_Rout, t_Hown], writes=[tps])
                            fw.op("pe", lambda e, g=g, oc=oc: e.matmul(ps[:, oc], lhsT=Rout[:, g, 1, :], rhs=Hown[:, 1, g, :], start=False, stop=False),
                                  reads=[t_Rout, t_Hown], writes=[tps])
                            fw.op("pe", lambda e, g=g, oc=oc: e.matmul(ps[:, oc], lhsT=Mm[:, g, :], rhs=U2own[:, g, :], start=False, stop=True),
                                  reads=[t_Mm, t_U2], writes=[tps])
                        fw.op("dve", lambda e: e.tensor_tensor(out=Y2[:, g4 * 4:g4 * 4 + 4, :], in0=Y2[:, g4 * 4:g4 * 4 + 4, :],
                                                               in1=ps[:, :].rearrange("p (g n) -> p g n", n=128), op=ALU.add),
                              reads=[tps, t_Y2], writes=[t_Y2])
                    ro()

        s5_dir(0)
        s5_dir(1)
        fw.dma("sp", y2_d.rearrange("l h g n -> (l h) g n"), Y2[:], reads=[t_Y2], writes=[t_y2])
        if "s5" in debug:
            dd = dout("dbg_y2", [8, 16, 64, 128])
            t = Tok()
            fw.dma("sp", dd, y2_d, reads=[t_y2], writes=[t])
            final.append(t)

    x1_d = fw.dram("x1_d", [NOWN, D])
    t_x1 = Tok()
    GC = 2.0 * math.sqrt(2.0 / math.pi)
    with fw.phase():
        merged = fw.sb([128, KT, NOWN], F32R, "merged")
        t_mg = Tok()
        with fw.phase():
            sgT = fw.sb([128, 8, NOWN], F32R, "sgT")
            t_sgT = Tok()
            alloc_wblk(8)
            with fw.phase():
                tT = fw.sb([128, 8, NOWN], F32R, "tT")
                t_tT = Tok()
                yt = [fw.sb([128, 8, 128], F32, f"yt{i}") for i in range(2)]
                t_yt = [Tok(), Tok()]
                ga = fw.sb([128, NOWN], F32, "ga"); t_ga = Tok()
                gb_ = fw.sb([128, NOWN], F32, "gb"); t_gb = Tok()
                for mt in range(8):
                    def gelu_tile(mt=mt):
                        i = mt % 2
                        for gl in range(8):
                            fw.dma("sp", yt[i][gl * 16:(gl + 1) * 16, :, :], y2_d[:, :, mt * 8 + gl, :].rearrange("l h n -> h l n"),
                                   writes=[t_yt[i]], acc=(gl > 0))
                        xv = yt[i][:].rearrange("p l n -> p n l")
                        g3 = ga[:].rearrange("p (n l) -> p n l", l=8)
                        h3 = gb_[:].rearrange("p (n l) -> p n l", l=8)
                        fw.op("act", lambda e: e.activation(out=g3, in_=xv, func=AF.Square), reads=[t_yt[i]], writes=[t_ga])
                        fw.op("dve", lambda e: e.tensor_scalar(out=ga[:], in0=ga[:], scalar1=0.044715, scalar2=1.0, op0=ALU.mult, op1=ALU.add),
                              reads=[t_ga], writes=[t_ga])
                        fw.op("dve", lambda e: e.tensor_tensor(out=g3, in0=g3, in1=xv, op=ALU.mult), reads=[t_ga, t_yt[i]], writes=[t_ga])
                        fw.op("act", lambda e: e.activation(out=gb_[:], in_=ga[:], func=AF.Sigmoid, scale=GC), reads=[t_ga], writes=[t_gb])
                        fw.op("dve", lambda e: e.tensor_tensor(out=tT[:, mt, :].rearrange("p (n l) -> p n l", l=8), in0=h3, in1=xv, op=ALU.mult),
                              reads=[t_gb, t_yt[i]], writes=[t_tT], acc=True)
                    gelu_tile()
                for cb in range(2):
                    wb, twb = load_w(glu_w, cb * 512, 512, kt=8)
                    for m in range(4):
                        for c0 in (0, 512):
                            def glu_blk(wb=wb, twb=twb, m=m, c0=c0, cb=cb):
                                ps, tps = next_ps()
                                for k in range(8):
                                    fw.op("pe", lambda e, k=k: e.matmul(ps[:, :], lhsT=wb[:, k, m * 128:(m + 1) * 128], rhs=tT[:, k, c0:c0 + 512],
                                                                        start=(k == 0), stop=(k == 7)), reads=[twb, t_tT], writes=[tps])
                                fw.op("act", lambda e: e.activation(out=ga[:, 0:512], in_=ps[:, :], func=AF.Sigmoid), reads=[tps], writes=[t_ga])
                                fw.op("dve", lambda e: e.tensor_tensor(out=sgT[:, cb * 4 + m, c0:c0 + 512], in0=ga[:, 0:512],
                                                                       in1=tT[:, cb * 4 + m, c0:c0 + 512].bitcast(F32), op=ALU.mult),
                                      reads=[t_ga, t_tT], writes=[t_sgT], acc=True)
                            glu_blk()
                if "sg" in debug:
                    dd = dout("dbg_sgT", [1024, NOWN])
                    t = Tok()
                    fw.dma("sp", dd.rearrange("(k p) n -> p k n", p=128), sgT[:].bitcast(F32), reads=[t_sgT], writes=[t])
                    final.append(t)
            with fw.phase():
                ogT = fw.sb([128, 8, NOWN], F32R, "ogT")
                t_ogT = Tok()
                for k in range(8):
                    fw.dma("sp", ogT[:, k, :], ogT_d[k * 128:(k + 1) * 128, :].bitcast(F32R), writes=[t_ogT], acc=(k > 0))
                gt = [fw.sb([128, 512], F32, f"gt{i}") for i in range(4)]
                t_gt = [Tok() for _ in range(4)]
                t1 = [fw.sb([128, 512], F32, f"t1_{i}") for i in range(2)]
                t_t1 = [Tok(), Tok()]
                cnt = [0]
                for cb in range(4):
                    wa, twa = load_w(w_s5p, cb * 512, 512, kt=8)
                    wbb, twbb = load_w(w_glap, cb * 512, 512, kt=8)
                    for m in range(4):
                        for c0 in (0, 512):
                            def mg_blk(wa=wa, twa=twa, wbb=wbb, twbb=twbb, m=m, c0=c0, cb=cb):
                                j = cnt[0] % 2
                                cnt[0] += 1
                                mt = cb * 4 + m
                                pa, tpa = next_ps()
                                for k in range(8):
                                    fw.op("pe", lambda e, k=k: e.matmul(pa[:, :], lhsT=wa[:, k, m * 128:(m + 1) * 128], rhs=sgT[:, k, c0:c0 + 512],
                                                                        start=(k == 0), stop=(k == 7)), reads=[twa, t_sgT], writes=[tpa])
                                pb, tpb = next_ps()
                                for k in range(8):
                                    fw.op("pe", lambda e, k=k: e.matmul(pb[:, :], lhsT=wbb[:, k, m * 128:(m + 1) * 128], rhs=ogT[:, k, c0:c0 + 512],
                                                                        start=(k == 0), stop=(k == 7)), reads=[twbb, t_ogT], writes=[tpb])
                                fw.dma("sp", gt[2 * j][:], sgsT_d[mt * 128:(mt + 1) * 128, c0:c0 + 512], writes=[t_gt[2 * j]])
                                fw.dma("sp", gt[2 * j + 1][:], sggT_d[mt * 128:(mt + 1) * 128, c0:c0 + 512], writes=[t_gt[2 * j + 1]])
                                fw.op("dve", lambda e: e.tensor_tensor(out=t1[j][:], in0=pa[:, :], in1=gt[2 * j][:], op=ALU.mult),
                                      reads=[tpa, t_gt[2 * j]], writes=[t_t1[j]])
                                fw.op("dve", lambda e: e.tensor_tensor(out=gt[2 * j + 1][:], in0=pb[:, :], in1=gt[2 * j + 1][:], op=ALU.mult),
                                      reads=[tpb, t_gt[2 * j + 1]], writes=[t_gt[2 * j + 1]])
                                fw.op("dve", lambda e: e.tensor_tensor(out=merged[:, mt, c0:c0 + 512], in0=t1[j][:], in1=gt[2 * j + 1][:], op=ALU.add),
                                      reads=[t_t1[j], t_gt[2 * j + 1]], writes=[t_mg], acc=True)
                            mg_blk()
        fw.barrier()
        with fw.phase():
            alloc_wblk(KT)
            g1b = fw.sb([128, 512], F32, "g1b"); t_g1b = Tok()
            stg2 = [fw.sb([128, 512], F32, f"stg2_{i}") for i in range(3)]
            t_stg2 = [Tok() for _ in range(3)]
            s2i = [0]

            def next_stg2():
                i = s2i[0]
                s2i[0] = (i + 1) % 3
                return stg2[i], t_stg2[i]
            xs = [fw.sb([128, 512], F32, f"xs{i}") for i in range(2)]
            t_xs = [Tok(), Tok()]
            cnt2 = [0]
            for cb in range(4):
                wb, twb = load_w(w_out, cb * 512, 512)
                fw.dma("sp", g1b[:], mod_d[0:1, 2 * D + cb * 512:2 * D + (cb + 1) * 512].to_broadcast([128, 512]), writes=[t_g1b])
                for ti in range(8):
                    def wo_blk(wb=wb, twb=twb, ti=ti, cb=cb):
                        j = cnt2[0] % 2
                        cnt2[0] += 1
                        ps, tps = next_ps()
                        for k in range(KT):
                            fw.op("pe", lambda e, k=k: e.matmul(ps[:, :], lhsT=merged[:, k, ti * 128:(ti + 1) * 128], rhs=wb[:, k, :],
                                                                start=(k == 0), stop=(k == KT - 1)), reads=[twb, t_mg], writes=[tps])
                        fw.dma("sp", xs[j][:], x_own[ti * 128:(ti + 1) * 128, cb * 512:(cb + 1) * 512], writes=[t_xs[j]])
                        sg, tsg = next_stg2()
                        fw.op("dve", lambda e: e.tensor_tensor(out=sg[:], in0=ps[:, :], in1=g1b[:], op=ALU.mult), reads=[tps, t_g1b], writes=[tsg])
                        fw.op("dve", lambda e: e.tensor_tensor(out=sg[:], in0=sg[:], in1=xs[j][:], op=ALU.add), reads=[t_xs[j], tsg], writes=[tsg])
                        fw.dma("sp", x1_d[ti * 128:(ti + 1) * 128, cb * 512:(cb + 1) * 512], sg[:], reads=[tsg], writes=[t_x1], acc=True)
                    wo_blk()
        if "x1" in debug:
            dd = dout("dbg_x1", [NOWN, D])
            t = Tok()
            fw.dma("sp", dd, x1_d, reads=[t_x1], writes=[t])
            final.append(t)

    CAP = 512
    U32 = mybir.dt.uint32
    xg_d = fw.dram("xg_d", [64 * CAP, D])
    yg_d = fw.dram("yg_d", [64 * CAP, D])
    ysh_d = fw.dram("ysh_d", [NOWN, D])
    t_xg, t_yg2, t_ysh = Tok(), Tok(), Tok()
    with fw.phase():
        IDX = fw.sb([128, 8, 8], U32, "IDX")
        WK = fw.sb([128, 8, 8], F32, "WK")
        t_IDX = Tok()
        with fw.phase():
            h2T = fw.sb([128, KT, NOWN], F32R, "h2T")
            t_h2 = Tok()
            with fw.phase():
                srow = fw.sb([128, D], F32, "srow")
                shrow = fw.sb([128, D], F32, "shrow")
                t_row = Tok()
                fw.dma("sp", shrow[:], n2row.to_broadcast([128, D]), writes=[t_row])
                fw.dma("sp", srow[:], mod_d[0:1, 4 * D:5 * D].to_broadcast([128, D]), writes=[t_row], acc=True)
                fw.op("dve", lambda e: e.scalar_tensor_tensor(out=srow[:], in0=srow[:], scalar=1.0, in1=shrow[:], op0=ALU.add, op1=ALU.mult),
                      reads=[t_row], writes=[t_row])
                fw.dma("sp", shrow[:], mod_d[0:1, 3 * D:4 * D].to_broadcast([128, D]), reads=[t_row], writes=[t_row])
                xt2 = [fw.sb([128, D], F32, f"xt2_{i}") for i in range(2)]
                t_xt2 = [Tok(), Tok()]
                junk2 = fw.sb([128, D], BF16, "junk2")
                st2 = fw.sb([128, 4], F32, "st2")
                t_st2 = Tok()
                rw = fw.sb([128, KT, 64], F32R, "rw")
                t_rw = Tok()
                for k in range(KT):
                    fw.dma("sp", rw[:, k, :], router_w[k * 128:(k + 1) * 128, :].bitcast(F32R), writes=[t_rw], acc=(k > 0))
                rb = fw.sb([128, 64], F32, "rb")
                fw.dma("sp", rb[:], router_b.to_broadcast([128, 64]), writes=[t_rw], acc=True)
                tri = fw.sb([128, 128], F32, "tri")
                ones2 = fw.sb([128, 128], F32, "ones2")
                ioti = fw.sb([128, 64], mybir.dt.int32, "ioti")
                iotf = fw.sb([128, 64], F32, "iotf")
                t_cst = Tok()
                fw.op("pool", lambda e: e.memset(ones2[:], 1.0), writes=[t_cst])
                fw.op("pool", lambda e: e.affine_select(out=tri[:], in_=ones2[:], pattern=[[1, 128]], compare_op=ALU.is_gt, fill=0.0,
                                                        base=0, channel_multiplier=-1), reads=[t_cst], writes=[t_cst])
                fw.op("pool", lambda e: e.iota(out=ioti[:], pattern=[[CAP, 64]], base=0, channel_multiplier=0), reads=[t_cst], writes=[t_cst])
                fw.op("dve", lambda e: e.tensor_copy(out=iotf[:], in_=ioti[:]), reads=[t_cst], writes=[t_cst])
                SEL = fw.sb([128, 8, 64], F32, "SEL")
                t_SEL = Tok()
                NR = 16
                rt = fw.sb([128, NR, 64], F32, "rt")
                t_rt = Tok()

                def Rr(i):
                    return rt[:, i, :]

                def R3(i):
                    return rt[:, i, :].rearrange("p (a b) -> p a b", b=8)

                def g8(i):
                    return rt[:, i, 0:8]

                def rop(eng, fn, extra=()):
                    fw.op(eng, fn, reads=[t_rt, t_rw, t_cst] + list(extra), writes=[t_rt])

                for ti in range(8):
                    def n2_tile(ti=ti):
                        i = ti % 2
                        fw.dma("sp", xt2[i][:], x1_d[ti * 128:(ti + 1) * 128, :], writes=[t_xt2[i]])
                        fw.op("act", lambda e: e.activation(out=junk2[:], in_=xt2[i][:], func=AF.Square, accum_out=st2[:, 0:1]),
                              reads=[t_xt2[i], t_st2], writes=[t_st2])
                        fw.op("act", lambda e: e.activation(out=st2[:, 1:2], in_=st2[:, 0:1], func=AF.Sqrt, scale=1.0 / D, bias=EPS),
                              reads=[t_st2], writes=[t_st2])
                        fw.op("dve", lambda e: e.reciprocal(out=st2[:, 2:3], in_=st2[:, 1:2]), reads=[t_st2], writes=[t_st2])
                        fw.op("dve", lambda e: e.scalar_tensor_tensor(out=xt2[i][:], in0=xt2[i][:], scalar=st2[:, 2:3], in1=srow[:], op0=ALU.mult, op1=ALU.mult),
                              reads=[t_st2, t_xt2[i], t_row], writes=[t_xt2[i]])
                        fw.op("dve", lambda e: e.tensor_tensor(out=xt2[i][:], in0=xt2[i][:], in1=shrow[:], op=ALU.add),
                              reads=[t_xt2[i], t_row], writes=[t_xt2[i]])
                        for kb in range(4):
                            def kblk(kb=kb):
                                ps, tps = next_ps()
                                for kk in range(4):
                                    k = kb * 4 + kk
                                    fw.op("pe", lambda e, k=k, kk=kk: e.transpose(out=ps[:, kk * 128:(kk + 1) * 128],
                                                                                 in_=xt2[i][:, k * 128:(k + 1) * 128], identity=ident[:]),
                                          reads=[t_xt2[i], t_ident], writes=[tps])
                                fw.op("act", lambda e: e.activation(out=h2T[:, kb * 4:(kb + 1) * 4, ti * 128:(ti + 1) * 128],
                                                                    in_=ps[:, :].rearrange("p (k n) -> p k n", n=128), func=AF.Identity),
                                      reads=[tps], writes=[t_h2], acc=True)
                            kblk()
                        ps, tps = next_ps()
                        for k in range(KT):
                            fw.op("pe", lambda e, k=k: e.matmul(ps[:, 0:64], lhsT=h2T[:, k, ti * 128:(ti + 1) * 128], rhs=rw[:, k, :],
                                                                start=(k == 0), stop=(k == KT - 1)), reads=[t_rw, t_h2], writes=[tps])
                        fw.op("act", lambda e: e.activation(out=Rr(0), in_=ps[:, 0:64], func=AF.Sigmoid), reads=[tps, t_rt], writes=[t_rt])
                        rop("dve", lambda e: e.tensor_tensor(out=Rr(1), in0=Rr(0), in1=rb[:], op=ALU.add))
                        rop("dve", lambda e: e.tensor_reduce(out=g8(2), in_=R3(1), axis=AX.X, op=ALU.max))
                        rop("dve", lambda e: e.tensor_tensor(out=R3(3), in0=R3(1), in1=g8(2).unsqueeze(2).to_broadcast([128, 8, 8]), op=ALU.is_equal))
                        rop("dve", lambda e: e.scalar_tensor_tensor(out=Rr(3), in0=Rr(3), scalar=-1.0e4, in1=Rr(1), op0=ALU.mult, op1=ALU.add))
                        rop("dve", lambda e: e.tensor_reduce(out=g8(4), in_=R3(3), axis=AX.X, op=ALU.max))
                        rop("dve", lambda e: e.tensor_tensor(out=g8(5), in0=g8(2), in1=g8(4), op=ALU.add))
                        rop("dve", lambda e: e.max(out=g8(6), in_=g8(5)))
                        rop("dve", lambda e: e.tensor_scalar(out=g8(7), in0=g8(5), scalar1=rt[:, 6, 3:4], scalar2=None, op0=ALU.is_ge))
                        rop("dve", lambda e: e.tensor_scalar(out=g8(8), in0=g8(7), scalar1=1.0e4, scalar2=-1.0e4, op0=ALU.mult, op1=ALU.add))
                        rop("dve", lambda e: e.tensor_tensor(out=R3(9), in0=R3(1), in1=g8(7).unsqueeze(2).to_broadcast([128, 8, 8]), op=ALU.mult))
                        rop("dve", lambda e: e.tensor_tensor(out=R3(9), in0=R3(9), in1=g8(8).unsqueeze(2).to_broadcast([128, 8, 8]), op=ALU.add))
                        rop("dve", lambda e: e.max(out=g8(10), in_=Rr(9)))
                        fw.op("dve", lambda e: e.tensor_scalar(out=SEL[:, ti, :], in0=Rr(9), scalar1=rt[:, 10, 7:8], scalar2=None, op0=ALU.is_ge),
                              reads=[t_rt], writes=[t_SEL], acc=True)
                        rop("dve", lambda e: e.tensor_tensor(out=Rr(11), in0=SEL[:, ti, :], in1=Rr(0), op=ALU.mult), extra=[t_SEL])
                        rop("dve", lambda e: e.tensor_reduce(out=rt[:, 2, 0:1], in_=Rr(11), axis=AX.X, op=ALU.add))
                        rop("dve", lambda e: e.reciprocal(out=rt[:, 2, 1:2], in_=rt[:, 2, 0:1]))
                        rop("dve", lambda e: e.tensor_scalar(out=Rr(11), in0=Rr(11), scalar1=rt[:, 2, 1:2], scalar2=2.5, op0=ALU.mult, op1=ALU.mult))
                        pp, tpp = next_ps()
                        fw.op("pe", lambda e: e.matmul(pp[:, 0:64], lhsT=tri[:], rhs=SEL[:, ti, :], start=True, stop=(ti == 0)),
                              reads=[t_cst, t_SEL], writes=[tpp])
                        for tj in range(ti):
                            fw.op("pe", lambda e, tj=tj: e.matmul(pp[:, 0:64], lhsT=ones2[:], rhs=SEL[:, tj, :], start=False, stop=(tj == ti - 1)),
                                  reads=[t_cst, t_SEL], writes=[tpp])
                        rop("dve", lambda e: e.tensor_scalar(out=Rr(12), in0=pp[:, 0:64], scalar1=float(CAP - 1), scalar2=None, op0=ALU.min), extra=[tpp])
                        rop("dve", lambda e: e.tensor_tensor(out=Rr(12), in0=Rr(12), in1=iotf[:], op=ALU.add))
                        for k in range(8):
                            rop("dve", lambda e, k=k: e.tensor_scalar(out=Rr(13), in0=Rr(9), scalar1=rt[:, 10, k:k + 1], scalar2=None, op0=ALU.is_equal))
                            rop("dve", lambda e: e.tensor_tensor(out=Rr(14), in0=Rr(13), in1=Rr(12), op=ALU.mult))
                            rop("dve", lambda e, k=k: e.tensor_reduce(out=rt[:, 15, k:k + 1], in_=Rr(14), axis=AX.X, op=ALU.add))
                            rop("dve", lambda e: e.tensor_tensor(out=Rr(14), in0=Rr(13), in1=Rr(11), op=ALU.mult))
                            fw.op("dve", lambda e, k=k: e.tensor_reduce(out=WK[:, ti, k:k + 1], in_=Rr(14), axis=AX.X, op=ALU.add),
                                  reads=[t_rt], writes=[t_IDX], acc=True)
                        fw.op("dve", lambda e: e.tensor_copy(out=IDX[:, ti, :], in_=rt[:, 15, 0:8]), reads=[t_rt], writes=[t_IDX], acc=True)
                        for k in range(8):
                            fw.idma(xg_d, bass.IndirectOffsetOnAxis(ap=IDX[:, ti, k:k + 1], axis=0), xt2[i][:], None,
                                    reads=[t_xt2[i], t_IDX], writes=[t_xg], acc=True)
                    n2_tile()
            fw.barrier()
            if "rt" in debug:
                dd = dout("dbg_IDX", [128, 64], U32)
                t = Tok()
                fw.dma("sp", dd, IDX[:].rearrange("p t k -> p (t k)"), writes=[t])
                final.append(t)
                dd = dout("dbg_WK", [128, 64])
                t = Tok()
                fw.dma("sp", dd, WK[:].rearrange("p t k -> p (t k)"), writes=[t])
                final.append(t)
            with fw.phase():
                alloc_wblk(KT)
                Xe = fw.sb([128, 2, D], F32, "Xe")
                t_Xe = [Tok(), Tok()]
                XeT = fw.sb([128, KT, CAP], F32R, "XeT")
                t_XeT = Tok()
                hid = fw.sb([128, 4, NOWN], F32R, "hid")
                t_hid = Tok()
                Ye = [fw.sb([128, D], F32, f"Ye{i}") for i in range(2)]
                t_Ye = [Tok(), Tok()]
                yei = [0]

                def expert(ex):
                    shared = ex == 64
                    Wg = sh_g if shared else exp_g[ex]
                    Wu = sh_u if shared else exp_u[ex]
                    Wd = sh_d if shared else exp_d[ex]
                    ntk = NOWN if shared else CAP
                    wg, twg = load_w(Wg, 0, 512)
                    wu, twu = load_w(Wu, 0, 512)
                    if shared:
                        src, t_src = h2T, t_h2
                    else:
                        for st_ in range(CAP // 128):
                            xb = st_ % 2
                            fw.dma("sp", Xe[:, xb, :], xg_d[ex * CAP + st_ * 128:ex * CAP + (st_ + 1) * 128, :], reads=[t_xg], writes=[t_Xe[xb]])
                            for kb in range(4):
                                def xt_blk(st_=st_, kb=kb, xb=xb):
                                    ps, tps = next_ps()
                                    for kk in range(4):
                                        k = kb * 4 + kk
                                        fw.op("pe", lambda e, k=k, kk=kk: e.transpose(out=ps[:, kk * 128:(kk + 1) * 128],
                                                                                     in_=Xe[:, xb, k * 128:(k + 1) * 128], identity=ident[:]),
                                              reads=[t_Xe[xb], t_ident], writes=[tps])
                                    fw.op("act", lambda e: e.activation(out=XeT[:, kb * 4:(kb + 1) * 4, st_ * 128:(st_ + 1) * 128],
                                                                        in_=ps[:, :].rearrange("p (k n) -> p k n", n=128), func=AF.Identity),
                                          reads=[tps], writes=[t_XeT], acc=(st_ + kb > 0))
                                xt_blk()
                        src, t_src = XeT, t_XeT
                    cw = 512
                    for f in range(4):
                        for c0 in range(0, ntk, cw):
                            def gu(f=f, c0=c0):
                                pg, tpg = next_ps()
                                for k in range(KT):
                                    fw.op("pe", lambda e, k=k: e.matmul(pg[:, 0:cw], lhsT=wg[:, k, f * 128:(f + 1) * 128], rhs=src[:, k, c0:c0 + cw],
                                                                        start=(k == 0), stop=(k == KT - 1)), reads=[twg, t_src], writes=[tpg])
                                pu, tpu = next_ps()
                                for k in range(KT):
                                    fw.op("pe", lambda e, k=k: e.matmul(pu[:, 0:cw], lhsT=wu[:, k, f * 128:(f + 1) * 128], rhs=src[:, k, c0:c0 + cw],
                                                                        start=(k == 0), stop=(k == KT - 1)), reads=[twu, t_src], writes=[tpu])
                                fw.op("act", lambda e: e.activation(out=hid[:, f, c0:c0 + cw], in_=pg[:, 0:cw], func=AF.Silu), reads=[tpg], writes=[t_hid],
                                      acc=(f + c0 > 0))
                                fw.op("dve", lambda e: e.tensor_tensor(out=hid[:, f, c0:c0 + cw], in0=hid[:, f, c0:c0 + cw].bitcast(F32), in1=pu[:, 0:cw],
                                                                       op=ALU.mult), reads=[tpu, t_hid], writes=[t_hid], acc=True)
                            gu()
                    wblk, t_wblk = WB["buf"], WB["tok"]
                    bi = wbi[0]
                    wbi[0] = (bi + 1) % NWB
                    wd = wblk[bi][:].rearrange("p k n -> p (k n)").rearrange("p (k n) -> p k n", k=4)
                    twd = t_wblk[bi]
                    for k in range(4):
                        fw.dma("sp", wd[:, k, :], Wd[k * 128:(k + 1) * 128, :].bitcast(F32R), writes=[twd], acc=(k > 0))
                    for ti in range(ntk // 128):
                        def dn_tile(ti=ti):
                            j = yei[0]
                            yei[0] = 1 - j
                            for db in range(4):
                                def dn(db=db):
                                    pd, tpd = next_ps()
                                    for f in range(4):
                                        fw.op("pe", lambda e, f=f: e.matmul(pd[:, :], lhsT=hid[:, f, ti * 128:(ti + 1) * 128],
                                                                            rhs=wd[:, f, db * 512:(db + 1) * 512], start=(f == 0), stop=(f == 3)),
                                              reads=[twd, t_hid], writes=[tpd])
                                    if db % 2 == 0:
                                        fw.op("act", lambda e: e.activation(out=Ye[j][:, db * 512:(db + 1) * 512], in_=pd[:, :], func=AF.Identity),
                                              reads=[tpd], writes=[t_Ye[j]], acc=(db > 0))
                                    else:
                                        fw.op("dve", lambda e: e.tensor_copy(out=Ye[j][:, db * 512:(db + 1) * 512], in_=pd[:, :]),
                                              reads=[tpd], writes=[t_Ye[j]], acc=True)
                                dn()
                            if shared:
                                fw.dma("sp", ysh_d[ti * 128:(ti + 1) * 128, :], Ye[j][:], reads=[t_Ye[j]], writes=[t_ysh], acc=True)
                            else:
                                fw.dma("sp", yg_d[ex * CAP + ti * 128:ex * CAP + (ti + 1) * 128, :], Ye[j][:], reads=[t_Ye[j]], writes=[t_yg2], acc=True)
                        dn_tile()

                NEXP = 0 if "noexp" in debug else 64
                for ex in range(NEXP):
                    expert(ex)
                expert(64)
        fw.barrier()
        with fw.phase():
            g2b = fw.sb([128, D], F32, "g2b")
            fnb = fw.sb([128, D], F32, "fnb")
            t_gb2 = Tok()
            fw.dma("sp", g2b[:], mod_d[0:1, 5 * D:6 * D].to_broadcast([128, D]), writes=[t_gb2])
            fw.dma("sp", fnb[:], fnw.to_broadcast([128, D]), writes=[t_gb2], acc=True)
            xf = [fw.sb([128, D], F32, f"xf{i}") for i in range(2)]
            t_xf = [Tok(), Tok()]
            Am = [fw.sb([128, D], F32, f"Am{i}") for i in range(2)]
            t_Am = [Tok(), Tok()]
            Gb = [fw.sb([128, D], F32, f"Gb{i}") for i in range(3)]
            t_Gb = [Tok() for _ in range(3)]
            gbi = [0]
            junk3 = fw.sb([128, D], BF16, "junk3")
            st3 = fw.sb([128, 4], F32, "st3")
            t_st3 = Tok()
            for ti in range(8):
                def fin(ti=ti):
                    i = ti % 2
                    fw.dma("sp", xf[i][:], x1_d[ti * 128:(ti + 1) * 128, :], writes=[t_xf[i]])
                    fw.dma("sp", Am[i][:], ysh_d[ti * 128:(ti + 1) * 128, :], writes=[t_Am[i]])
                    for k in range(8):
                        def comb(k=k):
                            g = gbi[0]
                            gbi[0] = (g + 1) % 3
                            fw.idma(Gb[g][:], None, yg_d, bass.IndirectOffsetOnAxis(ap=IDX[:, ti, k:k + 1], axis=0), reads=[], writes=[t_Gb[g]])
                            fw.op("dve", lambda e: e.scalar_tensor_tensor(out=Am[i][:], in0=Gb[g][:], scalar=WK[:, ti, k:k + 1], in1=Am[i][:],
                                                                          op0=ALU.mult, op1=ALU.add), reads=[t_Gb[g], t_Am[i]], writes=[t_Am[i]])
                        comb()
                    if "moe" in debug:
                        dd = dout(f"dbg_moe{ti}", [128, D])
                        t = Tok()
                        fw.dma("sp", dd, Am[i][:], reads=[t_Am[i]], writes=[t])
                        final.append(t)
                    fw.op("dve", lambda e: e.tensor_tensor(out=Am[i][:], in0=Am[i][:], in1=g2b[:], op=ALU.mult), reads=[t_gb2, t_Am[i]], writes=[t_Am[i]])
                    fw.op("dve", lambda e: e.tensor_tensor(out=xf[i][:], in0=xf[i][:], in1=Am[i][:], op=ALU.add),
                          reads=[t_Am[i], t_xf[i]], writes=[t_xf[i]])
                    fw.op("act", lambda e: e.activation(out=junk3[:], in_=xf[i][:], func=AF.Square, accum_out=st3[:, 0:1]),
                          reads=[t_xf[i], t_st3], writes=[t_st3])
                    fw.op("act", lambda e: e.activation(out=st3[:, 1:2], in_=st3[:, 0:1], func=AF.Sqrt, scale=1.0 / D, bias=EPS),
                          reads=[t_st3], writes=[t_st3])
                    fw.op("dve", lambda e: e.reciprocal(out=st3[:, 2:3], in_=st3[:, 1:2]), reads=[t_st3], writes=[t_st3])
                    fw.op("dve", lambda e: e.scalar_tensor_tensor(out=xf[i][:], in0=xf[i][:], scalar=st3[:, 2:3], in1=fnb[:], op0=ALU.mult, op1=ALU.mult),
                          reads=[t_st3, t_xf[i], t_gb2], writes=[t_xf[i]])
                    t = Tok()
                    fw.dma("sp", out[ti * 128:(ti + 1) * 128, :], xf[i][:], reads=[t_xf[i]], writes=[t])
                    final.append(t)
                fin()

    fw.finish(final)


def _prep_core(inputs, b, j):
    rev = (j == 1)
    xb = inputs["x"][b]
    cb = inputs["ctx"][b]
    if rev:
        xb = xb[::-1]
        cb = cb[::-1]
    dA = 1 if rev else 0
    dB = 1 - dA
    cc = np.stack([inputs["c"][b], inputs["c_ctx"]], axis=-1)
    w_in = inputs["w_in"][0]
    m = {
        "x_own": np.ascontiguousarray(xb[0:NOWN]),
        "x_oth": np.ascontiguousarray(xb[NOWN:2 * NOWN]),
        "x_ctx": np.ascontiguousarray(cb),
        "cT": np.ascontiguousarray(cc.reshape(KT, 128, 2).transpose(1, 0, 2)),
        "ada_w": inputs["ada_w"][0],
        "ada_b": inputs["ada_b"][0][None, :],
        "n1w": np.ascontiguousarray(inputs["norm1_w"][0].reshape(KT, 128).T),
        "w_in": w_in,
        "w_gk": np.ascontiguousarray(np.concatenate([w_in[:, 4096 + 16 * dA:4096 + 16 * dA + 16],
                                                     w_in[:, 4096 + 16 * dB:4096 + 16 * dB + 16],
                                                     np.zeros((D, 96), np.float32)], axis=1)),
        "gk_up": np.ascontiguousarray(inputs["gla_gk_up"][0][[dA, dB]]),
        "gk_b": np.ascontiguousarray(inputs["gla_gk_b"][0][[dA, dB]][:, None, :]),
        "gla_nw": np.ascontiguousarray(inputs["gla_norm_w"][0].reshape(2, 128).T),
        "s5_l": np.ascontiguousarray(np.stack([np.stack([inputs["s5_lam_re"][0][d].T, inputs["s5_lam_im"][0][d].T,
                                                          np.broadcast_to(inputs["s5_log_dt"][0][d][None, :], (64, 64))]) for d in (dA, dB)])),
        "s5_bc": np.ascontiguousarray(np.stack([np.stack([inputs["s5_b_re"][0][d].transpose(1, 0, 2).reshape(64, 1024),
                                                           inputs["s5_b_im"][0][d].transpose(1, 0, 2).reshape(64, 1024),
                                                           inputs["s5_c_re"][0][d].transpose(2, 0, 1).reshape(64, 1024),
                                                           inputs["s5_c_im"][0][d].transpose(2, 0, 1).reshape(64, 1024)]) for d in (dA, dB)])),
        "s5_dv": np.ascontiguousarray(np.tile(inputs["s5_d"][0].reshape(64, 16).T, (8, 1))),
        "glu_w": inputs["s5_glu_w"][0],
        "w_s5p": inputs["w_s5_proj"][0],
        "w_glap": inputs["w_gla_proj"][0],
        "w_out": inputs["w_out"][0],
        "n2w": np.ascontiguousarray(inputs["norm2_w"][0].reshape(KT, 128).T),
        "n2row": inputs["norm2_w"][0][None, :],
        "router_w": inputs["router_w"][0],
        "router_b": inputs["router_bias"][0][None, :],
        "exp_g": inputs["exp_w_gate"][0],
        "exp_u": inputs["exp_w_up"][0],
        "exp_d": inputs["exp_w_down"][0],
        "sh_g": inputs["sh_w_gate"][0],
        "sh_u": inputs["sh_w_up"][0],
        "sh_d": inputs["sh_w_down"][0],
        "fnw": inputs["final_norm_w"][None, :],
    }
    return m


_NC_CACHE = {}


def run(inputs, debug=()):
    inputs = {k: np.asarray(v) for k, v in inputs.items()}
    key = tuple(sorted(debug))
    if key not in _NC_CACHE:
        _NC_CACHE[key] = build(debug)
    nc = _NC_CACHE[key]
    in_maps = []
    for core in range(8):
        b, j = divmod(core, 2)
        in_maps.append(_prep_core(inputs, b, j))
    res = run_bass_kernel_spmd(nc, in_maps, core_ids=list(range(8)))
    return res.results


def kernel(**inputs):
    results = run(inputs)
    outp = np.zeros((4, 2048, D), np.float32)
    for core in range(8):
        b, j = divmod(core, 2)
        o = np.asarray(results[core]["out"])
        if j == 0:
            outp[b, 0:NOWN] = o
        else:
            outp[b, NOWN:] = o[::-1]
    return outp
```

```python
import contextlib
import math
import numpy as np
import concourse.bass as bass
import concourse.mybir as mybir
from concourse.bass_utils import run_bass_kernel_spmd

F32 = mybir.dt.float32
F32R = mybir.dt.float32r
BF16 = mybir.dt.bfloat16
AF = mybir.ActivationFunctionType
ALU = mybir.AluOpType
AX = mybir.AxisListType

D = 2048
KT = 16
NOWN = 1024
NCTX = 256
EPS = 1e-6
NRING = 24


class Tok:
    __slots__ = ("w", "r")

    def __init__(self):
        self.w = []
        self.r = {}


class FW:
    def __init__(self, nc, es):
        self.nc = nc
        self.es = es
        self.streams = {e: [] for e in ("pe", "act", "dve", "pool", "sp")}
        self.sem = {e: es.enter_context(nc.semaphore("s_" + e)) for e in ("pe", "act", "dve", "pool")}
        self.cnt = {e: 0 for e in self.sem}
        self.waited = {e: {} for e in self.streams}
        self.dring = [es.enter_context(nc.semaphore(f"dq{i}")) for i in range(NRING)]
        self.dcnt = [0] * NRING
        self.dnext = 0
        self.nsb = 0

    def sb(self, shape, dtype=F32, name=None):
        self.nsb += 1
        return self.es.enter_context(self.nc.sbuf_tensor(f"{name or 'sb'}_{self.nsb}", list(shape), dtype))

    def ps(self, shape, dtype=F32, name=None):
        self.nsb += 1
        return self.es.enter_context(self.nc.psum_tensor(name or f"ps{self.nsb}", list(shape), dtype))

    def dram(self, name, shape, dtype=F32, kind="Internal"):
        return self.nc.dram_tensor(name, list(shape), dtype, kind=kind).ap()

    def _wait(self, eng, deps):
        for (sem, val, src) in deps:
            if src is not None and src == eng and eng == "pe":
                continue
            key = id(sem)
            if self.waited[eng].get(key, 0) >= val:
                continue
            self.waited[eng][key] = val
            self.streams[eng].append(lambda e, sem=sem, val=val: e.wait_ge(sem, val))

    @staticmethod
    def _deps(reads, writes, acc=False):
        d = []
        for t in reads:
            d.extend(t.w)
        for t in writes:
            if not acc:
                d.extend(t.w)
            d.extend(t.r.values())
        return d

    @staticmethod
    def _mark(tok, reads, writes, key, acc=False):
        for t in reads:
            t.r[key] = tok
        for t in writes:
            if acc:
                t.w.append(tok)
            else:
                t.w = [tok]
            t.r = {}

    def op(self, eng, fn, reads=(), writes=(), acc=False):
        self._wait(eng, self._deps(reads, writes, acc))
        self.cnt[eng] += 1
        sem = self.sem[eng]
        self.streams[eng].append(lambda e, fn=fn, sem=sem: fn(e).then_inc(sem, 1))
        self._mark((sem, self.cnt[eng], eng), reads, writes, eng, acc)

    def dma(self, q, out, in_, reads=(), writes=(), acc=False, **kw):
        i = self.dnext
        self.dnext = (i + 1) % NRING
        sem = self.dring[i]
        deps = self._deps(reads, writes, acc)
        if self.dcnt[i]:
            deps.append((sem, self.dcnt[i], None))
        self._wait(q, deps)
        self.dcnt[i] += 16
        self.streams[q].append(lambda e, out=out, in_=in_, sem=sem, kw=kw: e.dma_start(out=out, in_=in_, **kw).then_inc(sem, 16))
        self._mark((sem, self.dcnt[i], None), reads, writes, ("d", i), acc)

    def idma(self, out, out_off, in_, in_off, reads=(), writes=(), acc=False):
        i = self.dnext
        self.dnext = (i + 1) % NRING
        sem = self.dring[i]
        deps = self._deps(reads, writes, acc)
        if self.dcnt[i]:
            deps.append((sem, self.dcnt[i], None))
        self._wait("pool", deps)
        self.dcnt[i] += 16
        self.streams["pool"].append(lambda e, out=out, in_=in_, sem=sem: e.indirect_dma_start(out, out_off, in_, in_off).then_inc(sem, 16))
        self._mark((sem, self.dcnt[i], None), reads, writes, ("d", i), acc)

    def barrier(self):
        engs = ("pe", "act", "dve", "pool")
        deps = [(self.sem[e], self.cnt[e], e) for e in engs if self.cnt[e]]
        deps += [(self.dring[i], self.dcnt[i], None) for i in range(NRING) if self.dcnt[i]]
        for e in engs + ("sp",):
            self._wait(e, deps)

    @contextlib.contextmanager
    def phase(self):
        old = self.es
        with contextlib.ExitStack() as es2:
            self.es = es2
            try:
                yield
            finally:
                self.barrier()
                self.es = old

    def finish(self, final_toks):
        deps = []
        for t in final_toks:
            deps.extend(t.w)
        self._wait("sp", deps)
        nc = self.nc
        with nc.Block() as block:
            @block.sync
            def _(e):
                for f in self.streams["sp"]:
                    f(e)

            @block.tensor
            def _(e):
                for f in self.streams["pe"]:
                    f(e)

            @block.scalar
            def _(e):
                for f in self.streams["act"]:
                    f(e)

            @block.vector
            def _(e):
                for f in self.streams["dve"]:
                    f(e)

            @block.gpsimd
            def _(e):
                for f in self.streams["pool"]:
                    f(e)


def r32(ap):
    return ap.bitcast(F32R)


def build(debug=()):
    nc = bass.Bass("TRN2", target_bir_lowering=False, dynamic_dma_scratch_size=4096)
    nc.dge_precook = False
    es = contextlib.ExitStack()
    with es:
        fw = FW(nc, es)
        _build(nc, fw, set(debug))
    return nc


def _build(nc, fw, debug):
    def din(name, shape, dtype=F32):
        return nc.dram_tensor(name, list(shape), dtype, kind="ExternalInput").ap()

    def dout(name, shape, dtype=F32):
        return nc.dram_tensor(name, list(shape), dtype, kind="ExternalOutput").ap()

    x_own = din("x_own", [NOWN, D])
    x_oth = din("x_oth", [NOWN, D])
    x_ctx = din("x_ctx", [NCTX, D])
    cT = din("cT", [128, KT, 2])
    ada_w = din("ada_w", [D, 6 * D])
    ada_b = din("ada_b", [1, 6 * D])
    n1w = din("n1w", [128, KT])
    w_in = din("w_in", [D, 8224])
    w_gk = din("w_gk", [D, 128])
    gk_up = din("gk_up", [2, 16, 512])
    gk_b = din("gk_b", [2, 1, 512])
    gla_nw = din("gla_nw", [128, 2])
    s5_l = din("s5_l", [2, 3, 64, 64])
    s5_bc = din("s5_bc", [2, 4, 64, 1024])
    s5_dv = din("s5_dv", [128, 64])
    glu_w = din("glu_w", [1024, 1024])
    w_s5p = din("w_s5p", [1024, D])
    w_glap = din("w_glap", [1024, D])
    w_out = din("w_out", [D, D])
    n2w = din("n2w", [128, KT])
    n2row = din("n2row", [1, D])
    router_w = din("router_w", [D, 64])
    router_b = din("router_b", [1, 64])
    exp_g = din("exp_g", [64, D, 512])
    exp_u = din("exp_u", [64, D, 512])
    exp_d = din("exp_d", [64, 512, D])
    sh_g = din("sh_g", [D, 512])
    sh_u = din("sh_u", [D, 512])
    sh_d = din("sh_d", [512, D])
    fnw = din("fnw", [1, D])

    out = dout("out", [NOWN, D])
    final = []

    ident = fw.sb([128, 128], F32, "ident")
    t_ident = Tok()
    fw.op("pool", lambda e: e.memset(ident[:], 0.0), writes=[t_ident])
    fw.op("pool", lambda e: e.affine_select(out=ident[:], in_=ident[:], pattern=[[-1, 128]], compare_op=ALU.not_equal,
                                            fill=1.0, base=0, channel_multiplier=1), reads=[t_ident], writes=[t_ident])

    psall = fw.ps([128, 8, 512], F32, "psall")
    psb = [psall[:, i, :] for i in range(8)]
    t_ps = [Tok() for _ in range(8)]
    psi = [0]

    def next_ps():
        i = psi[0]
        psi[0] = (i + 1) % 8
        return psb[i], t_ps[i]

    modT = fw.sb([128, 96, 2], F32, "modT")
    t_modT = Tok()
    s1 = fw.sb([128, KT, 2], F32, "s1")
    t_s1 = Tok()
    NWB = 2
    WB = {}
    wbi = [0]

    def alloc_wblk(kt=KT):
        WB["buf"] = [fw.sb([128, kt, 512], F32R, f"wblk{fw.nsb}_{i}") for i in range(NWB)]
        WB["tok"] = [Tok() for _ in range(NWB)]

    def load_w(src, col0, ncols, kt=KT):
        wblk, t_wblk = WB["buf"], WB["tok"]
        i = wbi[0]
        wbi[0] = (i + 1) % NWB
        v = src.rearrange("(k p) n -> p k n", p=128)
        for k in range(kt):
            fw.dma("sp", wblk[i][:, k, 0:ncols], src[k * 128:(k + 1) * 128, col0:col0 + ncols].bitcast(F32R), writes=[t_wblk[i]], acc=(k > 0))
        return wblk[i], t_wblk[i]

    mod_d = fw.dram("mod_d", [2, 6 * D])
    t_modd = Tok()
    with fw.phase():
        alloc_wblk()
        cs = fw.sb([128, KT, 128], F32R, "cs")
        t_cs = Tok()
        cin = fw.sb([128, KT, 2], F32, "cin")
        t_cin = Tok()
        for k in range(KT):
            fw.op("dve", lambda e, k=k: e.tensor_scalar(out=cs[:, k, :], in0=ident[:], scalar1=0.0, scalar2=None, op0=ALU.mult),
                  reads=[t_ident], writes=[t_cs])
        fw.dma("sp", cin[:], cT, writes=[t_cin])
        fw.op("act", lambda e: e.activation(out=cs[:, :, 0:2], in_=cin[:], func=AF.Silu), reads=[t_cin], writes=[t_cs])
        mblk = [fw.sb([2, 512], F32, f"mblk{i}") for i in range(2)]
        t_mblk = [Tok() for _ in range(2)]
        ablk = [fw.sb([2, 512], F32, f"ablk{i}") for i in range(2)]
        t_ablk = [Tok() for _ in range(2)]
        psT, tpsT = next_ps()
        for cb in range(24):
            wb, twb = load_w(ada_w, cb * 512, 512)
            i = cb % 2
            fw.dma("sp", ablk[i][0:1, :], ada_b[:, cb * 512:(cb + 1) * 512], writes=[t_ablk[i]])
            fw.dma("sp", ablk[i][1:2, :], ada_b[:, cb * 512:(cb + 1) * 512], writes=[t_ablk[i]])
            ps, tps = next_ps()
            if ps is psT:
                ps, tps = next_ps()
            for k in range(KT):
                fw.op("pe", lambda e, ps=ps, wb=wb, k=k: e.matmul(ps[:, :], lhsT=cs[:, k, :], rhs=wb[:, k, :],
                                                                   start=(k == 0), stop=(k == KT - 1)),
                      reads=[t_cs, twb], writes=[tps])
            fw.op("dve", lambda e, ps=ps, i=i: e.tensor_tensor(out=mblk[i][:], in0=ps[0:2, :], in1=ablk[i][:], op=ALU.add),
                  reads=[tps, t_ablk[i]], writes=[t_mblk[i]])
            fw.dma("act", mod_d[:, cb * 512:(cb + 1) * 512], mblk[i][:], reads=[t_mblk[i]], writes=[t_modd])
            for jj in range(4):
                j = cb * 4 + jj
                fw.op("pe", lambda e, i=i, j=j, jj=jj: e.transpose(out=psT[:, 2 * j:2 * j + 2], in_=mblk[i][0:2, jj * 128:(jj + 1) * 128],
                                                                    identity=ident[0:2, 0:2]),
                      reads=[t_mblk[i], t_ident], writes=[tpsT])
        fw.op("dve", lambda e: e.tensor_copy(out=modT[:].rearrange("p j r -> p (j r)"), in_=psT[:, 0:192]),
              reads=[tpsT], writes=[t_modT])
        n1 = fw.sb([128, KT], F32, "n1")
        t_n1 = Tok()
        fw.dma("sp", n1[:], n1w, writes=[t_n1])
        for r in range(2):
            fw.op("dve", lambda e, r=r: e.scalar_tensor_tensor(out=s1[:, :, r], in0=modT[:, KT:2 * KT, r], scalar=1.0, in1=n1[:],
                                                                op0=ALU.add, op1=ALU.mult),
                  reads=[t_modT, t_n1], writes=[t_s1])
        if "mod" in debug:
            dbg_modT = dout("dbg_modT", [128, 192])
            t = Tok()
            fw.dma("sp", dbg_modT, modT[:].rearrange("p j r -> p (j r)"), reads=[t_modT], writes=[t])
            final.append(t)

    with fw.phase():
        alloc_wblk()
        hT = fw.sb([128, KT, 1024], F32R, "hT")
        t_hT = Tok()
        xt = [fw.sb([128, D], F32, f"xt{i}") for i in range(2)]
        t_xt = [Tok() for _ in range(2)]
        junk = fw.sb([128, D], BF16, "junk")
        t_junk = Tok()
        st = fw.sb([128, 4], F32, "st")
        t_st = Tok()
        xti = [0]

        def norm_block(src, ntile, r):
            for ti in range(ntile):
                i = xti[0]
                xti[0] = 1 - i
                fw.dma("sp", xt[i][:], src[ti * 128:(ti + 1) * 128, :], writes=[t_xt[i]])
                fw.op("act", lambda e, i=i: e.activation(out=junk[:], in_=xt[i][:], func=AF.Square, accum_out=st[:, 0:1]),
                      reads=[t_xt[i]], writes=[t_junk, t_st])
                fw.op("act", lambda e: e.activation(out=st[:, 1:2], in_=st[:, 0:1], func=AF.Sqrt, scale=1.0 / D, bias=EPS),
                      reads=[t_st], writes=[t_st])
                fw.op("dve", lambda e: e.reciprocal(out=st[:, 2:3], in_=st[:, 1:2]), reads=[t_st], writes=[t_st])
                fw.op("dve", lambda e, i=i: e.tensor_scalar(out=xt[i][:], in0=xt[i][:], scalar1=st[:, 2:3], scalar2=None, op0=ALU.mult),
                      reads=[t_st, t_xt[i]], writes=[t_xt[i]])
                for kb in range(4):
                    ps, tps = next_ps()
                    for kk in range(4):
                        k = kb * 4 + kk
                        fw.op("pe", lambda e, ps=ps, i=i, k=k, kk=kk: e.transpose(out=ps[:, kk * 128:(kk + 1) * 128],
                                                                                  in_=xt[i][:, k * 128:(k + 1) * 128], identity=ident[:]),
                              reads=[t_xt[i], t_ident], writes=[tps])
                    for kk in range(4):
                        k = kb * 4 + kk
                        fw.op("act", lambda e, ps=ps, k=k, kk=kk, ti=ti: e.activation(out=hT[:, k, ti * 128:(ti + 1) * 128],
                                                                                      in_=ps[:, kk * 128:(kk + 1) * 128], func=AF.Identity,
                                                                                      scale=s1[:, k, r:r + 1], bias=modT[:, k, r:r + 1]),
                              reads=[tps, t_s1, t_modT], writes=[t_hT], acc=(ti + k > 0))

        NB = {"ctx": 32, "own": 128, "oth": 128}
        TOK0 = {"ctx": 0, "own": 256, "oth": 1280}
        u2_d = {b: fw.dram("u2_" + b, [8, 16, 64, NB[b]], BF16) for b in NB}
        qT_d = fw.dram("qT_d", [512, NOWN])
        kT_d = fw.dram("kT_d", [512, NOWN])
        ktok_d = fw.dram("ktok_d", [2304, 512])
        vtok_d = fw.dram("vtok_d", [2304, 1024])
        gkdT_d = fw.dram("gkdT_d", [32, 2304])
        sgoT_d = fw.dram("sgoT_d", [1024, NOWN])
        sgsT_d = fw.dram("sgsT_d", [2048, NOWN])
        sggT_d = fw.dram("sggT_d", [2048, NOWN])
        t_scr = Tok()

        NST = 4
        stg = [fw.sb([128, 512], F32, f"stg{i}") for i in range(NST)]
        t_stg = [Tok() for _ in range(NST)]
        stgb = [fw.sb([128, 8, 64], BF16, f"stgb{i}") for i in range(2)]
        t_stgb = [Tok() for _ in range(2)]
        sti = [0, 0]

        def next_stg():
            i = sti[0]
            sti[0] = (i + 1) % NST
            return stg[i], t_stg[i]

        def fm_linear(W, col0, ncols, ntok, evac):
            wb, twb = load_w(W, col0, ncols)
            for m in range(ncols // 128):
                for c0 in range(0, ntok, 512):
                    cw = min(512, ntok - c0)
                    ps, tps = next_ps()
                    for k in range(KT):
                        fw.op("pe", lambda e, ps=ps, wb=wb, k=k, m=m, c0=c0, cw=cw: e.matmul(
                            ps[:, 0:cw], lhsT=wb[:, k, m * 128:(m + 1) * 128], rhs=hT[:, k, c0:c0 + cw],
                            start=(k == 0), stop=(k == KT - 1)), reads=[twb, t_hT], writes=[tps])
                    evac(ps, tps, m, c0, cw)

        def tm_linear(W, col0, ncols, ntok, evac):
            wb, twb = load_w(W, col0, ncols)
            for ti in range(ntok // 128):
                ps, tps = next_ps()
                for k in range(KT):
                    fw.op("pe", lambda e, ps=ps, wb=wb, k=k, ti=ti: e.matmul(
                        ps[:, 0:ncols], lhsT=hT[:, k, ti * 128:(ti + 1) * 128], rhs=wb[:, k, 0:ncols],
                        start=(k == 0), stop=(k == KT - 1)), reads=[twb, t_hT], writes=[tps])
                evac(ps, tps, ti)

        def evac_to(dst, func=AF.Identity, scale=1.0, rows=128):
            def mk(ch0):
                def ev(ps, tps, m, c0, cw):
                    sg, tsg = next_stg()
                    fw.op("act", lambda e: e.activation(out=sg[0:rows, 0:cw], in_=ps[0:rows, 0:cw], func=func, scale=scale),
                          reads=[tps], writes=[tsg])
                    r0 = ch0 + m * 128
                    fw.dma("act", dst[r0:r0 + rows, c0:c0 + cw], sg[0:rows, 0:cw], reads=[tsg], writes=[t_scr], acc=True)
                return ev
            return mk

        def evac_u(blk):
            def mk(g0):
                def ev(ps, tps, m, c0, cw):
                    i = sti[1]
                    sti[1] = 1 - i
                    nn = cw // 8
                    fw.op("act", lambda e: e.activation(out=stgb[i][:, :, 0:nn],
                                                        in_=ps[:, 0:cw].rearrange("p (n s) -> p s n", s=8), func=AF.Identity),
                          reads=[tps], writes=[t_stgb[i]])
                    for gl in range(8):
                        g = g0 + m * 8 + gl
                        fw.dma("act", u2_d[blk][:, :, g, c0 // 8:c0 // 8 + nn].rearrange("s h n -> h s n"),
                               stgb[i][gl * 16:(gl + 1) * 16, :, 0:nn], reads=[t_stgb[i]], writes=[t_scr], acc=True)
                return ev
            return mk

        def evac_tok(dst, tok0, c0):
            def ev(ps, tps, ti):
                sg, tsg = next_stg()
                fw.op("dve", lambda e: e.tensor_copy(out=sg[:, :], in_=ps[:, :]), reads=[tps], writes=[tsg])
                fw.dma("act", dst[tok0 + ti * 128:tok0 + (ti + 1) * 128, c0:c0 + 512], sg[:, :], reads=[tsg], writes=[t_scr], acc=True)
            return ev

        def project(blk, src, ntile, r, full):
            ntok = ntile * 128
            norm_block(src, ntile, r)
            tok0 = TOK0[blk]
            for cb in range(2):
                fm_linear(w_in, cb * 512, 512, ntok, evac_u(blk)(cb * 32))
            for cb in range(2):
                tm_linear(w_in, 2048 + cb * 512, 512, ntok, evac_tok(vtok_d, tok0, cb * 512))
            tm_linear(w_in, 1536, 512, ntok, evac_tok(ktok_d, tok0, 0))
            fm_linear(w_gk, 0, 128, ntok, evac_to(gkdT_d[:, tok0:tok0 + ntok], rows=32)(0))
            if full:
                fm_linear(w_in, 1024, 512, ntok, evac_to(qT_d, scale=128.0 ** -0.5)(0))
                fm_linear(w_in, 1536, 512, ntok, evac_to(kT_d)(0))
                for cb in range(2):
                    fm_linear(w_in, 3072 + cb * 512, 512, ntok, evac_to(sgoT_d, func=AF.Silu)(cb * 512))
                for cb in range(4):
                    fm_linear(w_in, 4128 + cb * 512, 512, ntok, evac_to(sgsT_d, func=AF.Sigmoid)(cb * 512))
                for cb in range(4):
                    fm_linear(w_in, 6176 + cb * 512, 512, ntok, evac_to(sggT_d, func=AF.Sigmoid)(cb * 512))

        project("own", x_own, 8, 0, True)
        project("oth", x_oth, 8, 0, False)
        project("ctx", x_ctx, 2, 1, False)

        if "proj" in debug:
            for nm, src in (("qT", qT_d), ("kT", kT_d), ("ktok", ktok_d), ("vtok", vtok_d), ("gkdT", gkdT_d),
                            ("sgoT", sgoT_d), ("sgsT", sgsT_d), ("sggT", sggT_d), ("u2own", u2_d["own"]), ("u2ctx", u2_d["ctx"])):
                dd = dout("dbg_" + nm, list(src.shape), src.dtype)
                t = Tok()
                fw.dma("sp", dd, src, reads=[t_scr], writes=[t])
                final.append(t)

    ogT_d = fw.dram("ogT_d", [1024, NOWN])
    t_og = Tok()
    with fw.phase():
        def cmat(name):
            return fw.sb([128, 128], F32, name), Tok()
        BD, t_BD = cmat("BD")
        inclA, t_inclA = cmat("inclA")
        remA, t_remA = cmat("remA")
        inclB, t_inclB = cmat("inclB")
        remB, t_remB = cmat("remB")
        maskA, t_maskA = cmat("maskA")
        maskB, t_maskB = cmat("maskB")
        ones, t_ones = cmat("ones")
        fw.op("pool", lambda e: e.memset(ones[:], 1.0), writes=[t_ones])
        fw.op("pool", lambda e: e.memset(BD[:], 0.0), writes=[t_BD])
        fw.op("pool", lambda e: e.memset(BD[0:64, 0:64], -1.0 / 16), reads=[t_BD], writes=[t_BD])
        fw.op("pool", lambda e: e.memset(BD[64:128, 64:128], -1.0 / 16), reads=[t_BD], writes=[t_BD])
        fw.op("pool", lambda e: e.affine_select(out=inclA[:], in_=BD[:], pattern=[[1, 128]], compare_op=ALU.is_ge, fill=0.0,
                                                base=0, channel_multiplier=-1), reads=[t_BD], writes=[t_inclA])
        fw.op("pool", lambda e: e.affine_select(out=inclB[:], in_=BD[:], pattern=[[-1, 128]], compare_op=ALU.is_ge, fill=0.0,
                                                base=0, channel_multiplier=1), reads=[t_BD], writes=[t_inclB])
        fw.op("pool", lambda e: e.tensor_tensor(out=remA[:], in0=BD[:], in1=inclA[:], op=ALU.subtract), reads=[t_BD, t_inclA], writes=[t_remA])
        fw.op("pool", lambda e: e.tensor_tensor(out=remB[:], in0=BD[:], in1=inclB[:], op=ALU.subtract), reads=[t_BD, t_inclB], writes=[t_remB])
        fw.op("pool", lambda e: e.tensor_scalar(out=maskA[:], in0=inclA[:], scalar1=-16.0, scalar2=None, op0=ALU.mult), reads=[t_inclA], writes=[t_maskA])
        fw.op("pool", lambda e: e.tensor_scalar(out=maskB[:], in0=remA[:], scalar1=-16.0, scalar2=None, op0=ALU.mult), reads=[t_remA], writes=[t_maskB])
        INCL = [(inclA, t_inclA), (inclB, t_inclB)]
        REM = [(remA, t_remA), (remB, t_remB)]
        MASK = [(maskA, t_maskA), (maskB, t_maskB)]

        gksb = [fw.sb([16, 2304], F32, f"gksb{i}") for i in range(2)]
        t_gksb = [Tok(), Tok()]
        gup = [fw.sb([16, 512], F32, f"gup{i}") for i in range(2)]
        gb = [fw.sb([1, 512], F32, f"gb{i}") for i in range(2)]
        t_gp = Tok()
        for dr in range(2):
            fw.dma("sp", gksb[dr][:], gkdT_d[dr * 16:(dr + 1) * 16, :], writes=[t_gksb[dr]])
            fw.dma("sp", gup[dr][:], gk_up[dr], writes=[t_gp], acc=True)
            fw.dma("sp", gb[dr][:], gk_b[dr], writes=[t_gp], acc=True)
        gnw = fw.sb([128, 2], F32, "gnw")
        fw.dma("sp", gnw[:], gla_nw, writes=[t_gp], acc=True)

        ygT = fw.sb([128, 8, NOWN], F32, "ygT")
        t_yg = Tok()
        Sst = fw.sb([128, 4, 256], F32, "Sst")
        t_S = [Tok() for _ in range(4)]
        NLB = 2
        ktok = [fw.sb([128, 512], F32, f"ktok{i}") for i in range(NLB)]
        vtok = [fw.sb([128, 1024], F32, f"vtok{i}") for i in range(NLB)]
        qt = [fw.sb([128, 4, 128], F32, f"qt{i}") for i in range(NLB)]
        kt = [fw.sb([128, 4, 128], F32, f"kt{i}") for i in range(NLB)]
        t_ld = [Tok() for _ in range(NLB)]
        e1 = fw.sb([128, 512], F32, "e1"); t_e1 = Tok()
        Lt = fw.sb([128, 512], F32, "Lt"); t_L = Tok()
        er = fw.sb([128, 512], F32, "er"); t_er = Tok()
        kd = fw.sb([128, 512], F32, "kd"); t_kd = Tok()
        NHB = 2
        eb = [fw.sb([128, 128], F32, f"eb{i}") for i in range(NHB)]; t_eb = [Tok() for _ in range(NHB)]
        enb = [fw.sb([128, 128], F32, f"enb{i}") for i in range(NHB)]; t_enb = [Tok() for _ in range(NHB)]
        qb = [fw.sb([128, 128], F32, f"qb{i}") for i in range(NHB)]; t_qb = [Tok() for _ in range(NHB)]
        kb = [fw.sb([128, 128], F32, f"kb{i}") for i in range(NHB)]; t_kb = [Tok() for _ in range(NHB)]
        sm = [fw.sb([128, 128], F32, f"sm{i}") for i in range(NHB)]; t_sm = [Tok() for _ in range(NHB)]
        hbi = [0]
        lbi = [0]

        for h in range(4):
            fw.op("dve", lambda e, h=h: e.memset(Sst[:, h, :], 0.0), writes=[t_S[h]])

        def gla_tile(dr, row0, own_tok0, first_dir):
            own = own_tok0 is not None
            lb = lbi[0]
            lbi[0] = (lb + 1) % NLB
            fw.dma("sp", ktok[lb][:], ktok_d[row0:row0 + 128, :], writes=[t_ld[lb]])
            fw.dma("sp", vtok[lb][:], vtok_d[row0:row0 + 128, :], writes=[t_ld[lb]], acc=True)
            if own:
                fw.dma("sp", qt[lb][:], qT_d[:, own_tok0:own_tok0 + 128].rearrange("(h p) n -> p h n", p=128), writes=[t_ld[lb]], acc=True)
                fw.dma("sp", kt[lb][:], kT_d[:, own_tok0:own_tok0 + 128].rearrange("(h p) n -> p h n", p=128), writes=[t_ld[lb]], acc=True)
            tld = t_ld[lb]
            incl, t_incl = INCL[dr]
            rem, t_rem = REM[dr]
            msk, t_msk = MASK[dr]
            zps, tz = next_ps()
            fw.op("pe", lambda e: e.matmul(zps[:, :], lhsT=gksb[dr][:, row0:row0 + 128], rhs=gup[dr][:], start=True, stop=False),
                  reads=[t_gksb[dr], t_gp], writes=[tz])
            fw.op("pe", lambda e: e.matmul(zps[:, :], lhsT=ones[0:1, :], rhs=gb[dr][:], start=False, stop=True),
                  reads=[t_ones, t_gp], writes=[tz])
            fw.op("act", lambda e: e.activation(out=e1[:], in_=zps[:, :], func=AF.Exp, scale=-1.0), reads=[tz], writes=[t_e1])
            fw.op("act", lambda e: e.activation(out=Lt[:], in_=e1[:], func=AF.Ln, bias=1.0), reads=[t_e1], writes=[t_L])
            rps, tr = next_ps()
            fw.op("pe", lambda e: e.matmul(rps[:, :], lhsT=rem[:], rhs=Lt[:], start=True, stop=True), reads=[t_rem, t_L], writes=[tr])
            fw.op("act", lambda e: e.activation(out=er[:], in_=rps[:, :], func=AF.Exp), reads=[tr], writes=[t_er])
            fw.op("pool", lambda e: e.tensor_tensor(out=kd[:], in0=ktok[lb][:], in1=er[:], op=ALU.mult), reads=[tld, t_er], writes=[t_kd])
            corder = (0, 1) if dr == 0 else (1, 0)
            def head(h):
                hb = hbi[0]
                hbi[0] = (hb + 1) % NHB
                bps, tb = next_ps()
                fw.op("pe", lambda e: e.matmul(bps[:, 0:128], lhsT=Lt[:, h * 128:(h + 1) * 128], rhs=incl[:], start=True, stop=True),
                      reads=[t_L, t_incl], writes=[tb])
                fw.op("act", lambda e: e.activation(out=eb[hb][:], in_=bps[:, 0:128], func=AF.Exp), reads=[tb], writes=[t_eb[hb]])
                if own:
                    fw.op("act", lambda e: e.activation(out=enb[hb][:], in_=bps[:, 0:128], func=AF.Exp, scale=-1.0), reads=[tb], writes=[t_enb[hb]])
                    fw.op("pool", lambda e: e.tensor_tensor(out=qb[hb][:], in0=qt[lb][:, h, :], in1=eb[hb][:], op=ALU.mult),
                          reads=[tld, t_eb[hb]], writes=[t_qb[hb]])
                    fw.op("pool", lambda e: e.tensor_tensor(out=kb[hb][:], in0=kt[lb][:, h, :], in1=enb[hb][:], op=ALU.mult),
                          reads=[tld, t_enb[hb]], writes=[t_kb[hb]])
                    sps, tsc = next_ps()
                    fw.op("pe", lambda e: e.matmul(sps[:, 0:128], lhsT=kb[hb][:], rhs=qb[hb][:], start=True, stop=True),
                          reads=[t_kb[hb], t_qb[hb]], writes=[tsc])
                    fw.op("dve", lambda e: e.tensor_tensor(out=sm[hb][:], in0=sps[:, 0:128], in1=msk[:], op=ALU.mult),
                          reads=[tsc, t_msk], writes=[t_sm[hb]])
                def chunk(c):
                    cs_ = slice(c * 64, (c + 1) * 64)
                    if own:
                        ops_, to = next_ps()
                        for vt in range(2):
                            vcol = slice(h * 256 + vt * 128, h * 256 + (vt + 1) * 128)
                            oc = slice(vt * 64, (vt + 1) * 64)
                            fw.op("pe", lambda e, vcol=vcol, oc=oc: e.matmul(ops_[:, oc], lhsT=vtok[lb][cs_, vcol], rhs=sm[hb][cs_, cs_],
                                                                              start=True, stop=False), reads=[tld, t_sm[hb]], writes=[to])
                            fw.op("pe", lambda e, vt=vt, oc=oc: e.matmul(ops_[:, oc], lhsT=Sst[:, h, vt * 128:(vt + 1) * 128], rhs=qb[hb][:, cs_],
                                                                          start=False, stop=True), reads=[t_S[h], t_qb[hb]], writes=[to])
                        tk = slice(own_tok0 + c * 64, own_tok0 + (c + 1) * 64)
                        for vt in range(2):
                            oc = slice(vt * 64, (vt + 1) * 64)
                            if first_dir:
                                fw.op("act", lambda e, vt=vt, oc=oc: e.activation(out=ygT[:, h * 2 + vt, tk], in_=ops_[:, oc], func=AF.Identity),
                                      reads=[to], writes=[t_yg], acc=True)
                            else:
                                fw.op("dve", lambda e, vt=vt, oc=oc: e.tensor_tensor(out=ygT[:, h * 2 + vt, tk], in0=ygT[:, h * 2 + vt, tk],
                                                                                      in1=ops_[:, oc], op=ALU.add),
                                      reads=[to], writes=[t_yg], acc=True)
                    dps, td = next_ps()
                    fw.op("pe", lambda e: e.matmul(dps[:, 0:256], lhsT=kd[cs_, h * 128:(h + 1) * 128], rhs=vtok[lb][cs_, h * 256:(h + 1) * 256],
                                                   start=True, stop=True), reads=[t_kd, tld], writes=[td])
                    col = c * 64 + 63 if dr == 0 else c * 64
                    fw.op("dve", lambda e, col=col: e.scalar_tensor_tensor(out=Sst[:, h, :], in0=Sst[:, h, :], scalar=eb[hb][:, col:col + 1],
                                                                            in1=dps[:, 0:256], op0=ALU.mult, op1=ALU.add),
                          reads=[td, t_eb[hb], t_S[h]], writes=[t_S[h]])
                for c in corder:
                    chunk(c)
            for h in range(4):
                head(h)

        for t in range(2):
            gla_tile(0, t * 128, None, True)
        for t in range(8):
            gla_tile(0, 256 + t * 128, t * 128, True)
        fw.barrier()
        for h in range(4):
            fw.op("dve", lambda e, h=h: e.memset(Sst[:, h, :], 0.0), reads=[t_S[h]], writes=[t_S[h]])
        for t in (1, 0):
            gla_tile(1, t * 128, None, False)
        for t in range(7, -1, -1):
            gla_tile(1, 1280 + t * 128, None, False)
        for t in range(7, -1, -1):
            gla_tile(1, 256 + t * 128, t * 128, False)
        fw.barrier()

        if "yg" in debug:
            dd = dout("dbg_ygT", [1024, NOWN])
            t = Tok()
            fw.dma("sp", dd.rearrange("(k p) n -> p k n", p=128), ygT[:], writes=[t])
            final.append(t)

        sq = [fw.sb([128, 512], F32, f"sq{i}") for i in range(2)]; t_sq = [Tok(), Tok()]
        rs = fw.sb([128, 512], F32, "rs"); t_rs = Tok()
        sgo = [fw.sb([128, 512], F32, f"sgo{i}") for i in range(2)]; t_sgo = [Tok(), Tok()]
        ogs = [fw.sb([128, 512], F32, f"ogs{i}") for i in range(2)]; t_ogs = [Tok(), Tok()]
        def go_block(h, c0):
            if True:
                ssp, tss = next_ps()
                for vt in range(2):
                    fw.op("act", lambda e, vt=vt: e.activation(out=sq[vt][:], in_=ygT[:, h * 2 + vt, c0:c0 + 512], func=AF.Square),
                          writes=[t_sq[vt]])
                    fw.op("pe", lambda e, vt=vt: e.matmul(ssp[:, :], lhsT=ones[:], rhs=sq[vt][:], start=(vt == 0), stop=(vt == 1)),
                          reads=[t_ones, t_sq[vt]], writes=[tss])
                fw.op("act", lambda e: e.activation(out=rs[:], in_=ssp[:, :], func=AF.Sqrt, scale=1.0 / 256, bias=EPS), reads=[tss], writes=[t_rs])
                fw.op("dve", lambda e: e.reciprocal(out=rs[:], in_=rs[:]), reads=[t_rs], writes=[t_rs])
                for vt in range(2):
                    r0 = (h * 2 + vt) * 128
                    fw.dma("sp", sgo[vt][:], sgoT_d[r0:r0 + 128, c0:c0 + 512], writes=[t_sgo[vt]])
                    fw.op("dve", lambda e, vt=vt: e.tensor_tensor(out=ogs[vt][:], in0=ygT[:, h * 2 + vt, c0:c0 + 512], in1=rs[:], op=ALU.mult),
                          reads=[t_rs], writes=[t_ogs[vt]])
                    fw.op("dve", lambda e, vt=vt: e.scalar_tensor_tensor(out=ogs[vt][:], in0=ogs[vt][:], scalar=gnw[:, vt:vt + 1], in1=sgo[vt][:],
                                                                          op0=ALU.mult, op1=ALU.mult),
                          reads=[t_sgo[vt], t_gp, t_ogs[vt]], writes=[t_ogs[vt]])
                    fw.dma("sp", ogT_d[r0:r0 + 128, c0:c0 + 512], ogs[vt][:], reads=[t_ogs[vt]], writes=[t_og], acc=True)
        for h in range(4):
            for c0 in (0, 512):
                go_block(h, c0)
        if "og" in debug:
            dd = dout("dbg_ogT", [1024, NOWN])
            t = Tok()
            fw.dma("sp", dd, ogT_d, reads=[t_og], writes=[t])
            final.append(t)

    y2_d = fw.dram("y2_d", [8, 16, 64, 128])
    t_y2 = Tok()
    with fw.phase():
        import math
        U2own = fw.sb([128, 64, 128], BF16, "U2own")
        U2ctx = fw.sb([128, 64, 32], BF16, "U2ctx")
        t_U2 = Tok()
        for (dst, blk) in ((U2own, "own"), (U2ctx, "ctx")):
            fw.dma("sp", dst[:], u2_d[blk].rearrange("s h g n -> (s h) g n"), writes=[t_U2], acc=True)
        Y2 = fw.sb([128, 64, 128], F32, "Y2")
        t_Y2 = Tok()
        dv = fw.sb([128, 64], F32, "dv")
        t_dv = Tok()
        fw.dma("sp", dv[:], s5_dv, writes=[t_dv])
        fw.op("dve", lambda e: e.tensor_tensor(out=Y2[:], in0=U2own[:], in1=dv[:].unsqueeze(2).to_broadcast([128, 64, 128]), op=ALU.mult),
              reads=[t_U2, t_dv], writes=[t_Y2])
        mk = [fw.sb([128, 128], F32, f"mk{i}") for i in range(2)]
        t_mk = Tok()
        onesm = fw.sb([128, 128], F32, "onesm")
        fw.op("pool", lambda e: e.memset(onesm[:], 1.0), writes=[t_mk])
        fw.op("pool", lambda e: e.affine_select(out=mk[0][:], in_=onesm[:], pattern=[[16, 8], [0, 16]], compare_op=ALU.is_ge, fill=0.0,
                                                base=15, channel_multiplier=-1), reads=[t_mk], writes=[t_mk])
        fw.op("pool", lambda e: e.affine_select(out=mk[1][:], in_=onesm[:], pattern=[[-16, 8], [0, 16]], compare_op=ALU.is_ge, fill=0.0,
                                                base=0, channel_multiplier=1), reads=[t_mk], writes=[t_mk])
        Hown = fw.sb([64, 2, 64, 128], BF16, "Hown")
        t_Hown = Tok()
        GB = 8

        def tt(out, a, b, op, reads=(), writes=()):
            fw.op("dve", lambda e: e.tensor_tensor(out=out, in0=a, in1=b, op=op), reads=list(reads), writes=list(writes))

        def dve(fn, reads, writes):
            fw.op("dve", fn, reads=list(reads), writes=list(writes))

        def s5_dir(dr):
          with fw.phase():
            prm = fw.sb([64, 3, 64], F32, "prm")
            bc = fw.sb([64, 4, 1024], F32, "bcp")
            t_prm = Tok()
            sc = fw.sb([64, 16, 64], F32, "sc")
            t_sc = Tok()
            PW = fw.sb([64, 9, 2, 64], F32, "PW")
            t_PW = Tok()
            Bb = fw.sb([64, 2, 1024], F32, "Bb")
            t_Bb = Tok()
            LL = fw.sb([64, 4, 64], F32, "LL")
            t_LL = Tok()

            def S(i):
                return sc[:, i, :]
            fw.dma("sp", prm[:], s5_l[dr].rearrange("t p g -> p t g"), writes=[t_prm])
            fw.dma("sp", bc[:], s5_bc[dr].rearrange("t p f -> p t f"), writes=[t_prm], acc=True)
            R = [t_prm, t_sc]
            Wt = [t_sc]
            lre, lim, ldt = prm[:, 0, :], prm[:, 1, :], prm[:, 2, :]
            fw.op("act", lambda e: e.activation(out=S(0), in_=ldt, func=AF.Exp), reads=R, writes=Wt)
            dve(lambda e: e.scalar_tensor_tensor(out=S(1), in0=lre, scalar=1.0 / 16, in1=S(0), op0=ALU.mult, op1=ALU.mult), R, Wt)
            dve(lambda e: e.scalar_tensor_tensor(out=S(2), in0=lim, scalar=1.0 / 16, in1=S(0), op0=ALU.mult, op1=ALU.mult), R, Wt)
            fw.op("act", lambda e: e.activation(out=S(3), in_=S(1), func=AF.Exp), reads=R, writes=Wt)
            fw.op("act", lambda e: e.activation(out=S(4), in_=S(2), func=AF.Sin), reads=R, writes=Wt)
            fw.op("act", lambda e: e.activation(out=S(5), in_=S(2), func=AF.Sin, scale=-1.0, bias=math.pi / 2), reads=R, writes=Wt)
            tt(S(6), S(3), S(5), ALU.mult, R, Wt)
            tt(S(7), S(3), S(4), ALU.mult, R, Wt)
            for _ in range(4):
                tt(S(8), S(6), S(6), ALU.mult, R, Wt)
                tt(S(9), S(7), S(7), ALU.mult, R, Wt)
                dve(lambda e: e.scalar_tensor_tensor(out=S(7), in0=S(6), scalar=2.0, in1=S(7), op0=ALU.mult, op1=ALU.mult), R, Wt)
                tt(S(6), S(8), S(9), ALU.subtract, R, Wt)
            PWt = [t_PW]
            fw.op("dve", lambda e: e.memset(PW[:, 0, 0, :], 1.0), reads=[t_PW], writes=PWt)
            fw.op("dve", lambda e: e.memset(PW[:, 0, 1, :], 0.0), reads=[t_PW], writes=PWt)
            fw.op("dve", lambda e: e.tensor_copy(out=PW[:, 1, 0, :], in_=S(6)), reads=R + [t_PW], writes=PWt)
            fw.op("dve", lambda e: e.tensor_copy(out=PW[:, 1, 1, :], in_=S(7)), reads=R + [t_PW], writes=PWt)
            RR = R + [t_PW]
            for k in range(2, 9):
                tt(S(8), PW[:, k - 1, 0, :], S(6), ALU.mult, RR, Wt)
                tt(S(9), PW[:, k - 1, 1, :], S(7), ALU.mult, RR, Wt)
                tt(PW[:, k, 0, :], S(8), S(9), ALU.subtract, RR, PWt)
                tt(S(8), PW[:, k - 1, 0, :], S(7), ALU.mult, RR, Wt)
                tt(S(9), PW[:, k - 1, 1, :], S(6), ALU.mult, RR, Wt)
                tt(PW[:, k, 1, :], S(8), S(9), ALU.add, RR, PWt)
            tt(S(8), lre, lre, ALU.mult, RR, Wt)
            tt(S(9), lim, lim, ALU.mult, RR, Wt)
            tt(S(8), S(8), S(9), ALU.add, RR, Wt)
            dve(lambda e: e.reciprocal(out=S(10), in_=S(8)), RR, Wt)
            dve(lambda e: e.tensor_scalar(out=S(11), in0=S(6), scalar1=-1.0, scalar2=None, op0=ALU.add), RR, Wt)
            tt(S(8), S(11), lre, ALU.mult, RR, Wt)
            tt(S(9), S(7), lim, ALU.mult, RR, Wt)
            tt(S(8), S(8), S(9), ALU.add, RR, Wt)
            tt(S(12), S(8), S(10), ALU.mult, RR, Wt)
            tt(S(8), S(7), lre, ALU.mult, RR, Wt)
            tt(S(9), S(11), lim, ALU.mult, RR, Wt)
            tt(S(8), S(8), S(9), ALU.subtract, RR, Wt)
            tt(S(13), S(8), S(10), ALU.mult, RR, Wt)
            tt(S(8), PW[:, 7, 0, :], PW[:, 7, 0, :], ALU.mult, RR, Wt)
            tt(S(9), PW[:, 7, 1, :], PW[:, 7, 1, :], ALU.mult, RR, Wt)
            tt(S(8), S(8), S(9), ALU.add, RR, Wt)
            dve(lambda e: e.reciprocal(out=S(10), in_=S(8)), RR, Wt)
            tt(S(14), PW[:, 7, 0, :], S(10), ALU.mult, RR, Wt)
            dve(lambda e: e.scalar_tensor_tensor(out=S(15), in0=PW[:, 7, 1, :], scalar=-1.0, in1=S(10), op0=ALU.mult, op1=ALU.mult), RR, Wt)
            for q in range(2):
                fw.op("dve", lambda e, q=q: e.tensor_copy(out=LL[:, q, :], in_=PW[:, 8, 0, :]), reads=RR + [t_LL], writes=[t_LL])
            fw.op("dve", lambda e: e.tensor_scalar(out=LL[:, 2, :], in0=PW[:, 8, 1, :], scalar1=-1.0, scalar2=None, op0=ALU.mult), reads=RR + [t_LL], writes=[t_LL])
            fw.op("dve", lambda e: e.tensor_copy(out=LL[:, 3, :], in_=PW[:, 8, 1, :]), reads=RR + [t_LL], writes=[t_LL])

            if "s5p" in debug and dr == 0:
                for nm, src, shp, toks in (("PW", PW[:].rearrange("p k c g -> p (k c g)"), [64, 9 * 2 * 64], [t_PW]),
                                           ("LL", LL[:].rearrange("p k g -> p (k g)"), [64, 256], [t_LL]),
                                           ("sc", sc[:].rearrange("p k g -> p (k g)"), [64, 1024], [t_sc])):
                    dd = dout("dbg_" + nm, shp)
                    t = Tok()
                    fw.dma("sp", dd, src, reads=toks, writes=[t])
                    final.append(t)

            def v3(a):
                return a.rearrange("p (g h) -> p g h", h=16)

            def cmul(tmp, t_tmp, outr, outi, ar, ai, br, bi, G, neg_im=False, RD=(), WR=()):
                t0 = v3(tmp[:, 0, 0:G * 16])
                t1 = v3(tmp[:, 1, 0:G * 16])
                RDD = list(RD) + [t_tmp]
                tt(t0, br, ar, ALU.mult, RDD, [t_tmp])
                tt(t1, bi, ai, ALU.mult, RDD, [t_tmp])
                tt(outr, t0, t1, ALU.subtract, RDD + list(WR), list(WR))
                tt(t0, bi, ar, ALU.mult, RDD, [t_tmp])
                tt(t1, br, ai, ALU.mult, RDD, [t_tmp])
                if neg_im:
                    fw.op("dve", lambda e: e.scalar_tensor_tensor(out=outi, in0=t0, scalar=-1.0, in1=t1, op0=ALU.mult, op1=ALU.subtract),
                          reads=RDD + list(WR), writes=list(WR))
                else:
                    tt(outi, t0, t1, ALU.add, RDD + list(WR), list(WR))

            def bfull(a):
                return a.unsqueeze(2).to_broadcast([64, 64, 16])

            with fw.phase():
                tmpF = fw.sb([64, 2, 1024], F32, "tmpF")
                t_tmpF = Tok()
                cmul(tmpF, t_tmpF, v3(Bb[:, 0, :]), v3(Bb[:, 1, :]), bfull(S(12)), bfull(S(13)), v3(bc[:, 0, :]), v3(bc[:, 1, :]), 64,
                     RD=RR, WR=[t_Bb])
            RB = RR + [t_Bb]

            def pwb(k, c, gs_):
                return PW[:, k, c, gs_].unsqueeze(2).to_broadcast([64, GB, 16])

            def vb(a, gs_):
                return v3(a)[:, gs_, :]

            def gen_Wn(Wn, t_Wn, tmp, t_tmp, gs_):
                for j in range(8):
                    e_ = (7 - j) if dr == 0 else j
                    cmul(tmp, t_tmp, Wn[:, 0, :, j * 16:(j + 1) * 16], Wn[:, 1, :, j * 16:(j + 1) * 16], pwb(e_, 0, gs_), pwb(e_, 1, gs_),
                         vb(Bb[:, 0, :], gs_), vb(Bb[:, 1, :], gs_), GB, RD=RB, WR=[t_Wn])

            with fw.phase():
                Win = fw.sb([128, 64, 2, 64], BF16, "Win")
                t_Win = Tok()
                with fw.phase():
                    Wn = fw.sb([64, 2, GB, 128], F32, "Wn")
                    tmp = fw.sb([64, 2, GB * 16], F32, "tmp")
                    t_Wn, t_tmp = Tok(), Tok()
                    for gb in range(64 // GB):
                        g0 = gb * GB
                        gen_Wn(Wn, t_Wn, tmp, t_tmp, slice(g0, g0 + GB))
                        for g4 in range(GB // 4):
                            def blk4(g4=g4, g0=g0):
                                ps, tps = next_ps()
                                for gi in range(4):
                                    for c in range(2):
                                        fw.op("pe", lambda e, gi=gi, c=c: e.transpose(out=ps[:, (gi * 2 + c) * 64:(gi * 2 + c + 1) * 64],
                                                                                     in_=Wn[:, c, g4 * 4 + gi, :], identity=ident[0:64, 0:64]),
                                              reads=[t_Wn, t_ident], writes=[tps])
                                ga = g0 + g4 * 4
                                fw.op("act", lambda e: e.activation(out=Win[:, ga:ga + 4, :, :].rearrange("p g c q -> p (g c q)"), in_=ps[:, :],
                                                                    func=AF.Identity), reads=[tps], writes=[t_Win], acc=True)
                            blk4()
                if "s5p" in debug and dr == 0:
                    dd = dout("dbg_Win", [128, 64 * 2 * 64], BF16)
                    t = Tok()
                    fw.dma("sp", dd, Win[:].rearrange("p g c q -> p (g c q)"), reads=[t_Win], writes=[t])
                    final.append(t)
                    dd = dout("dbg_Bb", [64, 2048])
                    t = Tok()
                    fw.dma("sp", dd, Bb[:].rearrange("p c f -> p (c f)"), reads=[t_Bb], writes=[t])
                    final.append(t)
                with fw.phase():
                    E = fw.sb([64, 4, 64], F32, "E")
                    Pp = fw.sb([64, 4, 64], F32, "Pp")
                    Tt = fw.sb([64, 2, 64], F32, "Tt")
                    t_E, t_Pp, t_Tt = Tok(), Tok(), Tok()
                    fw.op("dve", lambda e: e.memset(E[:], 0.0), writes=[t_E])
                    if dr == 0:
                        blocks = [(U2ctx, n0, False) for n0 in range(0, 32, 8)] + [(U2own, n0, True) for n0 in range(0, 128, 8)]
                    else:
                        U2oth = fw.sb([128, 64, 128], BF16, "U2oth")
                        fw.dma("sp", U2oth[:], u2_d["oth"].rearrange("s h g n -> (s h) g n"), writes=[t_U2], acc=True)
                        blocks = [(U2ctx, n0, False) for n0 in range(24, -1, -8)] + [(U2oth, n0, False) for n0 in range(120, -1, -8)] + \
                                 [(U2own, n0, True) for n0 in range(120, -1, -8)]
                    pair = [0]

                    def scan_block(U2b, n0, own):
                        pi = pair[0]
                        pair[0] = (pi + 1) % 4
                        tp = [t_ps[2 * pi], t_ps[2 * pi + 1]]
                        Sp = psall[0:64, 2 * pi:2 * pi + 2, :]
                        for g in range(64):
                            for c in range(2):
                                fw.op("pe", lambda e, g=g, c=c: e.matmul(psall[0:64, 2 * pi + c, g * 8:(g + 1) * 8], lhsT=Win[:, g, c, :],
                                                                          rhs=U2b[:, g, n0:n0 + 8], start=True, stop=True),
                                      reads=[t_Win, t_U2], writes=[tp[c]])
                        S4 = Sp.rearrange("p c (g n) -> p c g n", n=8)
                        order = range(8) if dr == 0 else range(7, -1, -1)
                        for j in order:
                            if own:
                                fw.op("dve", lambda e, j=j: e.tensor_copy(out=Hown[:, :, :, n0 + j], in_=E[:, 0:2, :]), reads=[t_E], writes=[t_Hown], acc=True)
                            fw.op("dve", lambda e: e.tensor_tensor(out=Pp[:], in0=E[:], in1=LL[:], op=ALU.mult), reads=[t_E, t_LL, t_Pp], writes=[t_Pp])
                            fw.op("dve", lambda e: e.tensor_tensor(out=Tt[:], in0=Pp[:, 0:2, :], in1=Pp[:, 2:4, :], op=ALU.add), reads=[t_Pp, t_Tt], writes=[t_Tt])
                            fw.op("dve", lambda e, j=j: e.tensor_tensor(out=E[:, 0:2, :], in0=Tt[:], in1=S4[:, :, :, j], op=ALU.add),
                                  reads=[t_Tt, tp[0], tp[1], t_E], writes=[t_E])
                            fw.op("dve", lambda e: e.tensor_copy(out=E[:, 2:4, :], in_=E[:, 1::-1, :]), reads=[t_E], writes=[t_E])

                    for (U2b, n0, own) in blocks:
                        scan_block(U2b, n0, own)

            with fw.phase():
                Rout = fw.sb([64, 64, 2, 128], BF16, "Rout")
                Mm = fw.sb([128, 64, 128], BF16, "Mm")
                C7 = fw.sb([64, 2, 1024], F32, "C7")
                Wn3 = fw.sb([64, 2, GB, 128], F32, "Wn3")
                Rm = fw.sb([64, 2, GB, 128], F32, "Rm")
                tmp = fw.sb([64, 2, GB * 16], F32, "tmp2")
                t_Rout, t_Mm, t_C7, t_Wn, t_Rm, t_tmp = Tok(), Tok(), Tok(), Tok(), Tok(), Tok()
                with fw.phase():
                    tmpF = fw.sb([64, 2, 1024], F32, "tmpF2")
                    t_tmpF = Tok()
                    cmul(tmpF, t_tmpF, v3(C7[:, 0, :]), v3(C7[:, 1, :]), bfull(S(14)), bfull(S(15)), v3(bc[:, 2, :]), v3(bc[:, 3, :]), 64,
                         RD=RR, WR=[t_C7])
                RC = RB + [t_C7]
                for gb in range(64 // GB):
                    g0 = gb * GB
                    gs_ = slice(g0, g0 + GB)
                    gen_Wn(Wn3, t_Wn, tmp, t_tmp, gs_)
                    for j in range(8):
                        q_ = j if dr == 0 else (7 - j)
                        r_ = (j + 1) if dr == 0 else (8 - j)
                        cmul(tmp, t_tmp, Rm[:, 0, :, j * 16:(j + 1) * 16], Rm[:, 1, :, j * 16:(j + 1) * 16], pwb(q_, 0, gs_), pwb(q_, 1, gs_),
                             vb(C7[:, 0, :], gs_), vb(C7[:, 1, :], gs_), GB, neg_im=True, RD=RC, WR=[t_Rm])
                        cmul(tmp, t_tmp, Rout[:, gs_, 0, j * 16:(j + 1) * 16], Rout[:, gs_, 1, j * 16:(j + 1) * 16], pwb(r_, 0, gs_), pwb(r_, 1, gs_),
                             vb(bc[:, 2, :], gs_), vb(bc[:, 3, :], gs_), GB, neg_im=True, RD=RC, WR=[t_Rout])
                    for g4 in range(GB // 4):
                        def blkM(g4=g4, g0=g0):
                            ps2, tps2 = next_ps()
                            for gi in range(4):
                                for c in range(2):
                                    fw.op("pe", lambda e, gi=gi, c=c: e.matmul(ps2[:, gi * 128:(gi + 1) * 128], lhsT=Wn3[:, c, g4 * 4 + gi, :],
                                                                                rhs=Rm[:, c, g4 * 4 + gi, :], start=(c == 0), stop=(c == 1)),
                                          reads=[t_Wn, t_Rm], writes=[tps2])
                            ga = g0 + g4 * 4
                            fw.op("dve", lambda e: e.tensor_tensor(out=Mm[:, ga:ga + 4, :], in0=ps2[:, :].rearrange("p (g f) -> p g f", f=128),
                                                                   in1=mk[dr][:].unsqueeze(1).to_broadcast([128, 4, 128]), op=ALU.mult),
                                  reads=[tps2, t_mk], writes=[t_Mm], acc=True)
                        blkM()
                fw.barrier()
                if "s5p" in debug and dr == 0:
                    for nm, src, shp in (("Rout", Rout[:].rearrange("p g c f -> p (g c f)"), [64, 64 * 2 * 128]),
                                         ("Mm", Mm[:].rearrange("p g f -> p (g f)"), [128, 64 * 128]),
                                         ("Hown", Hown[:].rearrange("p c g n -> p (c g n)"), [64, 2 * 64 * 128])):
                        dd = dout("dbg_" + nm, shp, BF16)
                        t = Tok()
                        fw.dma("sp", dd, src, writes=[t])
                        final.append(t)
                for g4 in range(16):
                    def ro(g4=g4):
                        ps, tps = next_ps()
                        for gi in range(4):
                            g = g4 * 4 + gi
                            oc = slice(gi * 128, (gi + 1) * 128)
                            fw.op("pe", lambda e, g=g, oc=oc: e.matmul(ps[:, oc], lhsT=Rout[:, g, 0, :], rhs=Hown[:, 0, g, :], start=True, stop=False),
                                  reads=[t_Rout, t_Hown], writes=[tps])
                            fw.op("pe", lambda e, g=g, oc=oc: e.matmul(ps[:, oc], lhsT=Rout[:, g, 1, :], rhs=Hown[:, 1, g, :], start=False, stop=False),
                                  reads=[t_Rout, t_Hown], writes=[tps])
                            fw.op("pe", lambda e, g=g, oc=oc: e.matmul(ps[:, oc], lhsT=Mm[:, g, :], rhs=U2own[:, g, :], start=False, stop=True),
                                  reads=[t_Mm, t_U2], writes=[tps])
                        fw.op("dve", lambda e: e.tensor_tensor(out=Y2[:, g4 * 4:g4 * 4 + 4, :], in0=Y2[:, g4 * 4:g4 * 4 + 4, :],
                                                               in1=ps[:, :].rearrange("p (g n) -> p g n", n=128), op=ALU.add),
                              reads=[tps, t_Y2], writes=[t_Y2])
                    ro()

        s5_dir(0)
        s5_dir(1)
        fw.dma("sp", y2_d.rearrange("l h g n -> (l h) g n"), Y2[:], reads=[t_Y2], writes=[t_y2])
        if "s5" in debug:
            dd = dout("dbg_y2", [8, 16, 64, 128])
            t = Tok()
            fw.dma("sp", dd, y2_d, reads=[t_y2], writes=[t])
            final.append(t)

    x1_d = fw.dram("x1_d", [NOWN, D])
    t_x1 = Tok()
    GC = 2.0 * math.sqrt(2.0 / math.pi)
    with fw.phase():
        merged = fw.sb([128, KT, NOWN], F32R, "merged")
        t_mg = Tok()
        with fw.phase():
            sgT = fw.sb([128, 8, NOWN], F32R, "sgT")
            t_sgT = Tok()
            alloc_wblk(8)
            with fw.phase():
                tT = fw.sb([128, 8, NOWN], F32R, "tT")
                t_tT = Tok()
                yt = [fw.sb([128, 8, 128], F32, f"yt{i}") for i in range(2)]
                t_yt = [Tok(), Tok()]
                ga = fw.sb([128, NOWN], F32, "ga"); t_ga = Tok()
                gb_ = fw.sb([128, NOWN], F32, "gb"); t_gb = Tok()
                for mt in range(8):
                    def gelu_tile(mt=mt):
                        i = mt % 2
                        for gl in range(8):
                            fw.dma("sp", yt[i][gl * 16:(gl + 1) * 16, :, :], y2_d[:, :, mt * 8 + gl, :].rearrange("l h n -> h l n"),
                                   writes=[t_yt[i]], acc=(gl > 0))
                        xv = yt[i][:].rearrange("p l n -> p n l")
                        g3 = ga[:].rearrange("p (n l) -> p n l", l=8)
                        h3 = gb_[:].rearrange("p (n l) -> p n l", l=8)
                        fw.op("act", lambda e: e.activation(out=g3, in_=xv, func=AF.Square), reads=[t_yt[i]], writes=[t_ga])
                        fw.op("dve", lambda e: e.tensor_scalar(out=ga[:], in0=ga[:], scalar1=0.044715, scalar2=1.0, op0=ALU.mult, op1=ALU.add),
                              reads=[t_ga], writes=[t_ga])
                        fw.op("dve", lambda e: e.tensor_tensor(out=g3, in0=g3, in1=xv, op=ALU.mult), reads=[t_ga, t_yt[i]], writes=[t_ga])
                        fw.op("act", lambda e: e.activation(out=gb_[:], in_=ga[:], func=AF.Sigmoid, scale=GC), reads=[t_ga], writes=[t_gb])
                        fw.op("dve", lambda e: e.tensor_tensor(out=tT[:, mt, :].rearrange("p (n l) -> p n l", l=8), in0=h3, in1=xv, op=ALU.mult),
                              reads=[t_gb, t_yt[i]], writes=[t_tT], acc=True)
                    gelu_tile()
                for cb in range(2):
                    wb, twb = load_w(glu_w, cb * 512, 512, kt=8)
                    for m in range(4):
                        for c0 in (0, 512):
                            def glu_blk(wb=wb, twb=twb, m=m, c0=c0, cb=cb):
                                ps, tps = next_ps()
                                for k in range(8):
                                    fw.op("pe", lambda e, k=k: e.matmul(ps[:, :], lhsT=wb[:, k, m * 128:(m + 1) * 128], rhs=tT[:, k, c0:c0 + 512],
                                                                        start=(k == 0), stop=(k == 7)), reads=[twb, t_tT], writes=[tps])
                                fw.op("act", lambda e: e.activation(out=ga[:, 0:512], in_=ps[:, :], func=AF.Sigmoid), reads=[tps], writes=[t_ga])
                                fw.op("dve", lambda e: e.tensor_tensor(out=sgT[:, cb * 4 + m, c0:c0 + 512], in0=ga[:, 0:512],
                                                                       in1=tT[:, cb * 4 + m, c0:c0 + 512].bitcast(F32), op=ALU.mult),
                                      reads=[t_ga, t_tT], writes=[t_sgT], acc=True)
                            glu_blk()
                if "sg" in debug:
                    dd = dout("dbg_sgT", [1024, NOWN])
                    t = Tok()
                    fw.dma("sp", dd.rearrange("(k p) n -> p k n", p=128), sgT[:].bitcast(F32), reads=[t_sgT], writes=[t])
                    final.append(t)
            with fw.phase():
                ogT = fw.sb([128, 8, NOWN], F32R, "ogT")
                t_ogT = Tok()
                for k in range(8):
                    fw.dma("sp", ogT[:, k, :], ogT_d[k * 128:(k + 1) * 128, :].bitcast(F32R), writes=[t_ogT], acc=(k > 0))
                gt = [fw.sb([128, 512], F32, f"gt{i}") for i in range(4)]
                t_gt = [Tok() for _ in range(4)]
                t1 = [fw.sb([128, 512], F32, f"t1_{i}") for i in range(2)]
                t_t1 = [Tok(), Tok()]
                cnt = [0]
                for cb in range(4):
                    wa, twa = load_w(w_s5p, cb * 512, 512, kt=8)
                    wbb, twbb = load_w(w_glap, cb * 512, 512, kt=8)
                    for m in range(4):
                        for c0 in (0, 512):
                            def mg_blk(wa=wa, twa=twa, wbb=wbb, twbb=twbb, m=m, c0=c0, cb=cb):
                                j = cnt[0] % 2
                                cnt[0] += 1
                                mt = cb * 4 + m
                                pa, tpa = next_ps()
                                for k in range(8):
                                    fw.op("pe", lambda e, k=k: e.matmul(pa[:, :], lhsT=wa[:, k, m * 128:(m + 1) * 128], rhs=sgT[:, k, c0:c0 + 512],
                                                                        start=(k == 0), stop=(k == 7)), reads=[twa, t_sgT], writes=[tpa])
                                pb, tpb = next_ps()
                                for k in range(8):
                                    fw.op("pe", lambda e, k=k: e.matmul(pb[:, :], lhsT=wbb[:, k, m * 128:(m + 1) * 128], rhs=ogT[:, k, c0:c0 + 512],
                                                                        start=(k == 0), stop=(k == 7)), reads=[twbb, t_ogT], writes=[tpb])
                                fw.dma("sp", gt[2 * j][:], sgsT_d[mt * 128:(mt + 1) * 128, c0:c0 + 512], writes=[t_gt[2 * j]])
                                fw.dma("sp", gt[2 * j + 1][:], sggT_d[mt * 128:(mt + 1) * 128, c0:c0 + 512], writes=[t_gt[2 * j + 1]])
                                fw.op("dve", lambda e: e.tensor_tensor(out=t1[j][:], in0=pa[:, :], in1=gt[2 * j][:], op=ALU.mult),
                                      reads=[tpa, t_gt[2 * j]], writes=[t_t1[j]])
                                fw.op("dve", lambda e: e.tensor_tensor(out=gt[2 * j + 1][:], in0=pb[:, :], in1=gt[2 * j + 1][:], op=ALU.mult),
                                      reads=[tpb, t_gt[2 * j + 1]], writes=[t_gt[2 * j + 1]])
                                fw.op("dve", lambda e: e.tensor_tensor(out=merged[:, mt, c0:c0 + 512], in0=t1[j][:], in1=gt[2 * j + 1][:], op=ALU.add),
                                      reads=[t_t1[j], t_gt[2 * j + 1]], writes=[t_mg], acc=True)
                            mg_blk()
        fw.barrier()
        with fw.phase():
            alloc_wblk(KT)
            g1b = fw.sb([128, 512], F32, "g1b"); t_g1b = Tok()
            stg2 = [fw.sb([128, 512], F32, f"stg2_{i}") for i in range(3)]
            t_stg2 = [Tok() for _ in range(3)]
            s2i = [0]

            def next_stg2():
                i = s2i[0]
                s2i[0] = (i + 1) % 3
                return stg2[i], t_stg2[i]
            xs = [fw.sb([128, 512], F32, f"xs{i}") for i in range(2)]
            t_xs = [Tok(), Tok()]
            cnt2 = [0]
            for cb in range(4):
                wb, twb = load_w(w_out, cb * 512, 512)
                fw.dma("sp", g1b[:], mod_d[0:1, 2 * D + cb * 512:2 * D + (cb + 1) * 512].to_broadcast([128, 512]), writes=[t_g1b])
                for ti in range(8):
                    def wo_blk(wb=wb, twb=twb, ti=ti, cb=cb):
                        j = cnt2[0] % 2
                        cnt2[0] += 1
                        ps, tps = next_ps()
                        for k in range(KT):
                            fw.op("pe", lambda e, k=k: e.matmul(ps[:, :], lhsT=merged[:, k, ti * 128:(ti + 1) * 128], rhs=wb[:, k, :],
                                                                start=(k == 0), stop=(k == KT - 1)), reads=[twb, t_mg], writes=[tps])
                        fw.dma("sp", xs[j][:], x_own[ti * 128:(ti + 1) * 128, cb * 512:(cb + 1) * 512], writes=[t_xs[j]])
                        sg, tsg = next_stg2()
                        fw.op("dve", lambda e: e.tensor_tensor(out=sg[:], in0=ps[:, :], in1=g1b[:], op=ALU.mult), reads=[tps, t_g1b], writes=[tsg])
                        fw.op("dve", lambda e: e.tensor_tensor(out=sg[:], in0=sg[:], in1=xs[j][:], op=ALU.add), reads=[t_xs[j], tsg], writes=[tsg])
                        fw.dma("act", x1_d[ti * 128:(ti + 1) * 128, cb * 512:(cb + 1) * 512], sg[:], reads=[tsg], writes=[t_x1], acc=True)
                    wo_blk()
        if "x1" in debug:
            dd = dout("dbg_x1", [NOWN, D])
            t = Tok()
            fw.dma("sp", dd, x1_d, reads=[t_x1], writes=[t])
            final.append(t)

    CAP = 512
    U32 = mybir.dt.uint32
    xg_d = fw.dram("xg_d", [64 * CAP, D])
    yg_d = fw.dram("yg_d", [64 * CAP, D])
    ysh_d = fw.dram("ysh_d", [NOWN, D])
    t_xg, t_yg2, t_ysh = Tok(), Tok(), Tok()
    with fw.phase():
        IDX = fw.sb([128, 8, 8], U32, "IDX")
        WK = fw.sb([128, 8, 8], F32, "WK")
        t_IDX = Tok()
        with fw.phase():
            h2T = fw.sb([128, KT, NOWN], F32R, "h2T")
            t_h2 = Tok()
            with fw.phase():
                srow = fw.sb([128, D], F32, "srow")
                shrow = fw.sb([128, D], F32, "shrow")
                t_row = Tok()
                fw.dma("sp", shrow[:], n2row.to_broadcast([128, D]), writes=[t_row])
                fw.dma("sp", srow[:], mod_d[0:1, 4 * D:5 * D].to_broadcast([128, D]), writes=[t_row], acc=True)
                fw.op("dve", lambda e: e.scalar_tensor_tensor(out=srow[:], in0=srow[:], scalar=1.0, in1=shrow[:], op0=ALU.add, op1=ALU.mult),
                      reads=[t_row], writes=[t_row])
                fw.dma("sp", shrow[:], mod_d[0:1, 3 * D:4 * D].to_broadcast([128, D]), reads=[t_row], writes=[t_row])
                xt2 = [fw.sb([128, D], F32, f"xt2_{i}") for i in range(2)]
                t_xt2 = [Tok(), Tok()]
                junk2 = fw.sb([128, D], BF16, "junk2")
                st2 = fw.sb([128, 4], F32, "st2")
                t_st2 = Tok()
                rw = fw.sb([128, KT, 64], F32R, "rw")
                t_rw = Tok()
                for k in range(KT):
                    fw.dma("sp", rw[:, k, :], router_w[k * 128:(k + 1) * 128, :].bitcast(F32R), writes=[t_rw], acc=(k > 0))
                rb = fw.sb([128, 64], F32, "rb")
                fw.dma("sp", rb[:], router_b.to_broadcast([128, 64]), writes=[t_rw], acc=True)
                tri = fw.sb([128, 128], F32, "tri")
                ones2 = fw.sb([128, 128], F32, "ones2")
                ioti = fw.sb([128, 64], mybir.dt.int32, "ioti")
                iotf = fw.sb([128, 64], F32, "iotf")
                t_cst = Tok()
                fw.op("pool", lambda e: e.memset(ones2[:], 1.0), writes=[t_cst])
                fw.op("pool", lambda e: e.affine_select(out=tri[:], in_=ones2[:], pattern=[[1, 128]], compare_op=ALU.is_gt, fill=0.0,
                                                        base=0, channel_multiplier=-1), reads=[t_cst], writes=[t_cst])
                fw.op("pool", lambda e: e.iota(out=ioti[:], pattern=[[CAP, 64]], base=0, channel_multiplier=0), reads=[t_cst], writes=[t_cst])
                fw.op("dve", lambda e: e.tensor_copy(out=iotf[:], in_=ioti[:]), reads=[t_cst], writes=[t_cst])
                SEL = fw.sb([128, 8, 64], F32, "SEL")
                t_SEL = Tok()
                NR = 16
                rt = fw.sb([128, NR, 64], F32, "rt")
                t_rt = Tok()

                def Rr(i):
                    return rt[:, i, :]

                def R3(i):
                    return rt[:, i, :].rearrange("p (a b) -> p a b", b=8)

                def g8(i):
                    return rt[:, i, 0:8]

                def rop(eng, fn, extra=()):
                    fw.op(eng, fn, reads=[t_rt, t_rw, t_cst] + list(extra), writes=[t_rt])

                for ti in range(8):
                    def n2_tile(ti=ti):
                        i = ti % 2
                        fw.dma("sp", xt2[i][:], x1_d[ti * 128:(ti + 1) * 128, :], writes=[t_xt2[i]])
                        fw.op("act", lambda e: e.activation(out=junk2[:], in_=xt2[i][:], func=AF.Square, accum_out=st2[:, 0:1]),
                              reads=[t_xt2[i], t_st2], writes=[t_st2])
                        fw.op("act", lambda e: e.activation(out=st2[:, 1:2], in_=st2[:, 0:1], func=AF.Sqrt, scale=1.0 / D, bias=EPS),
                              reads=[t_st2], writes=[t_st2])
                        fw.op("dve", lambda e: e.reciprocal(out=st2[:, 2:3], in_=st2[:, 1:2]), reads=[t_st2], writes=[t_st2])
                        fw.op("dve", lambda e: e.scalar_tensor_tensor(out=xt2[i][:], in0=xt2[i][:], scalar=st2[:, 2:3], in1=srow[:], op0=ALU.mult, op1=ALU.mult),
                              reads=[t_st2, t_xt2[i], t_row], writes=[t_xt2[i]])
                        fw.op("dve", lambda e: e.tensor_tensor(out=xt2[i][:], in0=xt2[i][:], in1=shrow[:], op=ALU.add),
                              reads=[t_xt2[i], t_row], writes=[t_xt2[i]])
                        for kb in range(4):
                            def kblk(kb=kb):
                                ps, tps = next_ps()
                                for kk in range(4):
                                    k = kb * 4 + kk
                                    fw.op("pe", lambda e, k=k, kk=kk: e.transpose(out=ps[:, kk * 128:(kk + 1) * 128],
                                                                                 in_=xt2[i][:, k * 128:(k + 1) * 128], identity=ident[:]),
                                          reads=[t_xt2[i], t_ident], writes=[tps])
                                fw.op("act", lambda e: e.activation(out=h2T[:, kb * 4:(kb + 1) * 4, ti * 128:(ti + 1) * 128],
                                                                    in_=ps[:, :].rearrange("p (k n) -> p k n", n=128), func=AF.Identity),
                                      reads=[tps], writes=[t_h2], acc=True)
                            kblk()
                        ps, tps = next_ps()
                        for k in range(KT):
                            fw.op("pe", lambda e, k=k: e.matmul(ps[:, 0:64], lhsT=h2T[:, k, ti * 128:(ti + 1) * 128], rhs=rw[:, k, :],
                                                                start=(k == 0), stop=(k == KT - 1)), reads=[t_rw, t_h2], writes=[tps])
                        fw.op("act", lambda e: e.activation(out=Rr(0), in_=ps[:, 0:64], func=AF.Sigmoid), reads=[tps, t_rt], writes=[t_rt])
                        rop("dve", lambda e: e.tensor_tensor(out=Rr(1), in0=Rr(0), in1=rb[:], op=ALU.add))
                        rop("dve", lambda e: e.tensor_reduce(out=g8(2), in_=R3(1), axis=AX.X, op=ALU.max))
                        rop("dve", lambda e: e.tensor_tensor(out=R3(3), in0=R3(1), in1=g8(2).unsqueeze(2).to_broadcast([128, 8, 8]), op=ALU.is_equal))
                        rop("dve", lambda e: e.scalar_tensor_tensor(out=Rr(3), in0=Rr(3), scalar=-1.0e4, in1=Rr(1), op0=ALU.mult, op1=ALU.add))
                        rop("dve", lambda e: e.tensor_reduce(out=g8(4), in_=R3(3), axis=AX.X, op=ALU.max))
                        rop("dve", lambda e: e.tensor_tensor(out=g8(5), in0=g8(2), in1=g8(4), op=ALU.add))
                        rop("dve", lambda e: e.max(out=g8(6), in_=g8(5)))
                        rop("dve", lambda e: e.tensor_scalar(out=g8(7), in0=g8(5), scalar1=rt[:, 6, 3:4], scalar2=None, op0=ALU.is_ge))
                        rop("dve", lambda e: e.tensor_scalar(out=g8(8), in0=g8(7), scalar1=1.0e4, scalar2=-1.0e4, op0=ALU.mult, op1=ALU.add))
                        rop("dve", lambda e: e.tensor_tensor(out=R3(9), in0=R3(1), in1=g8(7).unsqueeze(2).to_broadcast([128, 8, 8]), op=ALU.mult))
                        rop("dve", lambda e: e.tensor_tensor(out=R3(9), in0=R3(9), in1=g8(8).unsqueeze(2).to_broadcast([128, 8, 8]), op=ALU.add))
                        rop("dve", lambda e: e.max(out=g8(10), in_=Rr(9)))
                        fw.op("dve", lambda e: e.tensor_scalar(out=SEL[:, ti, :], in0=Rr(9), scalar1=rt[:, 10, 7:8], scalar2=None, op0=ALU.is_ge),
                              reads=[t_rt], writes=[t_SEL], acc=True)
                        rop("dve", lambda e: e.tensor_tensor(out=Rr(11), in0=SEL[:, ti, :], in1=Rr(0), op=ALU.mult), extra=[t_SEL])
                        rop("dve", lambda e: e.tensor_reduce(out=rt[:, 2, 0:1], in_=Rr(11), axis=AX.X, op=ALU.add))
                        rop("dve", lambda e: e.reciprocal(out=rt[:, 2, 1:2], in_=rt[:, 2, 0:1]))
                        rop("dve", lambda e: e.tensor_scalar(out=Rr(11), in0=Rr(11), scalar1=rt[:, 2, 1:2], scalar2=2.5, op0=ALU.mult, op1=ALU.mult))
                        pp, tpp = next_ps()
                        fw.op("pe", lambda e: e.matmul(pp[:, 0:64], lhsT=tri[:], rhs=SEL[:, ti, :], start=True, stop=(ti == 0)),
                              reads=[t_cst, t_SEL], writes=[tpp])
                        for tj in range(ti):
                            fw.op("pe", lambda e, tj=tj: e.matmul(pp[:, 0:64], lhsT=ones2[:], rhs=SEL[:, tj, :], start=False, stop=(tj == ti - 1)),
                                  reads=[t_cst, t_SEL], writes=[tpp])
                        rop("dve", lambda e: e.tensor_scalar(out=Rr(12), in0=pp[:, 0:64], scalar1=float(CAP - 1), scalar2=None, op0=ALU.min), extra=[tpp])
                        rop("dve", lambda e: e.tensor_tensor(out=Rr(12), in0=Rr(12), in1=iotf[:], op=ALU.add))
                        for k in range(8):
                            rop("dve", lambda e, k=k: e.tensor_scalar(out=Rr(13), in0=Rr(9), scalar1=rt[:, 10, k:k + 1], scalar2=None, op0=ALU.is_equal))
                            rop("dve", lambda e: e.tensor_tensor(out=Rr(14), in0=Rr(13), in1=Rr(12), op=ALU.mult))
                            rop("dve", lambda e, k=k: e.tensor_reduce(out=rt[:, 15, k:k + 1], in_=Rr(14), axis=AX.X, op=ALU.add))
                            rop("dve", lambda e: e.tensor_tensor(out=Rr(14), in0=Rr(13), in1=Rr(11), op=ALU.mult))
                            fw.op("dve", lambda e, k=k: e.tensor_reduce(out=WK[:, ti, k:k + 1], in_=Rr(14), axis=AX.X, op=ALU.add),
                                  reads=[t_rt], writes=[t_IDX], acc=True)
                        fw.op("dve", lambda e: e.tensor_copy(out=IDX[:, ti, :], in_=rt[:, 15, 0:8]), reads=[t_rt], writes=[t_IDX], acc=True)
                        for k in range(8):
                            fw.idma(xg_d, bass.IndirectOffsetOnAxis(ap=IDX[:, ti, k:k + 1], axis=0), xt2[i][:], None,
                                    reads=[t_xt2[i], t_IDX], writes=[t_xg], acc=True)
                    n2_tile()
            fw.barrier()
            if "rt" in debug:
                dd = dout("dbg_IDX", [128, 64], U32)
                t = Tok()
                fw.dma("sp", dd, IDX[:].rearrange("p t k -> p (t k)"), writes=[t])
                final.append(t)
                dd = dout("dbg_WK", [128, 64])
                t = Tok()
                fw.dma("sp", dd, WK[:].rearrange("p t k -> p (t k)"), writes=[t])
                final.append(t)
            with fw.phase():
                alloc_wblk(KT)
                Xe = fw.sb([128, 2, D], F32, "Xe")
                t_Xe = [Tok(), Tok()]
                XeT = fw.sb([128, KT, CAP], F32R, "XeT")
                t_XeT = Tok()
                hid = fw.sb([128, 4, NOWN], F32R, "hid")
                t_hid = Tok()
                Ye = [fw.sb([128, D], F32, f"Ye{i}") for i in range(2)]
                t_Ye = [Tok(), Tok()]
                yei = [0]

                def expert(ex):
                    shared = ex == 64
                    Wg = sh_g if shared else exp_g[ex]
                    Wu = sh_u if shared else exp_u[ex]
                    Wd = sh_d if shared else exp_d[ex]
                    ntk = NOWN if shared else CAP
                    wg, twg = load_w(Wg, 0, 512)
                    wu, twu = load_w(Wu, 0, 512)
                    if shared:
                        src, t_src = h2T, t_h2
                    else:
                        for st_ in range(CAP // 128):
                            xb = st_ % 2
                            fw.dma("sp", Xe[:, xb, :], xg_d[ex * CAP + st_ * 128:ex * CAP + (st_ + 1) * 128, :], reads=[t_xg], writes=[t_Xe[xb]])
                            for kb in range(4):
                                def xt_blk(st_=st_, kb=kb, xb=xb):
                                    ps, tps = next_ps()
                                    for kk in range(4):
                                        k = kb * 4 + kk
                                        fw.op("pe", lambda e, k=k, kk=kk: e.transpose(out=ps[:, kk * 128:(kk + 1) * 128],
                                                                                     in_=Xe[:, xb, k * 128:(k + 1) * 128], identity=ident[:]),
                                              reads=[t_Xe[xb], t_ident], writes=[tps])
                                    fw.op("act", lambda e: e.activation(out=XeT[:, kb * 4:(kb + 1) * 4, st_ * 128:(st_ + 1) * 128],
                                                                        in_=ps[:, :].rearrange("p (k n) -> p k n", n=128), func=AF.Identity),
                                          reads=[tps], writes=[t_XeT], acc=(st_ + kb > 0))
                                xt_blk()
                        src, t_src = XeT, t_XeT
                    cw = 512
                    for f in range(4):
                        for c0 in range(0, ntk, cw):
                            def gu(f=f, c0=c0):
                                pg, tpg = next_ps()
                                for k in range(KT):
                                    fw.op("pe", lambda e, k=k: e.matmul(pg[:, 0:cw], lhsT=wg[:, k, f * 128:(f + 1) * 128], rhs=src[:, k, c0:c0 + cw],
                                                                        start=(k == 0), stop=(k == KT - 1)), reads=[twg, t_src], writes=[tpg])
                                pu, tpu = next_ps()
                                for k in range(KT):
                                    fw.op("pe", lambda e, k=k: e.matmul(pu[:, 0:cw], lhsT=wu[:, k, f * 128:(f + 1) * 128], rhs=src[:, k, c0:c0 + cw],
                                                                        start=(k == 0), stop=(k == KT - 1)), reads=[twu, t_src], writes=[tpu])
                                fw.op("act", lambda e: e.activation(out=hid[:, f, c0:c0 + cw], in_=pg[:, 0:cw], func=AF.Silu), reads=[tpg], writes=[t_hid],
                                      acc=(f + c0 > 0))
                                fw.op("dve", lambda e: e.tensor_tensor(out=hid[:, f, c0:c0 + cw], in0=hid[:, f, c0:c0 + cw].bitcast(F32), in1=pu[:, 0:cw],
                                                                       op=ALU.mult), reads=[tpu, t_hid], writes=[t_hid], acc=True)
                            gu()
                    wblk, t_wblk = WB["buf"], WB["tok"]
                    bi = wbi[0]
                    wbi[0] = (bi + 1) % NWB
                    wd = wblk[bi][:].rearrange("p k n -> p (k n)").rearrange("p (k n) -> p k n", k=4)
                    twd = t_wblk[bi]
                    for k in range(4):
                        fw.dma("sp", wd[:, k, :], Wd[k * 128:(k + 1) * 128, :].bitcast(F32R), writes=[twd], acc=(k > 0))
                    for ti in range(ntk // 128):
                        def dn_tile(ti=ti):
                            j = yei[0]
                            yei[0] = 1 - j
                            for db in range(4):
                                def dn(db=db):
                                    pd, tpd = next_ps()
                                    for f in range(4):
                                        fw.op("pe", lambda e, f=f: e.matmul(pd[:, :], lhsT=hid[:, f, ti * 128:(ti + 1) * 128],
                                                                            rhs=wd[:, f, db * 512:(db + 1) * 512], start=(f == 0), stop=(f == 3)),
                                              reads=[twd, t_hid], writes=[tpd])
                                    if db % 2 == 0:
                                        fw.op("act", lambda e: e.activation(out=Ye[j][:, db * 512:(db + 1) * 512], in_=pd[:, :], func=AF.Identity),
                                              reads=[tpd], writes=[t_Ye[j]], acc=(db > 0))
                                    else:
                                        fw.op("dve", lambda e: e.tensor_copy(out=Ye[j][:, db * 512:(db + 1) * 512], in_=pd[:, :]),
                                              reads=[tpd], writes=[t_Ye[j]], acc=True)
                                dn()
                            if shared:
                                fw.dma("act", ysh_d[ti * 128:(ti + 1) * 128, :], Ye[j][:], reads=[t_Ye[j]], writes=[t_ysh], acc=True)
                            else:
                                fw.dma("act", yg_d[ex * CAP + ti * 128:ex * CAP + (ti + 1) * 128, :], Ye[j][:], reads=[t_Ye[j]], writes=[t_yg2], acc=True)
                        dn_tile()

                NEXP = 0 if "noexp" in debug else 64
                for ex in range(NEXP):
                    expert(ex)
                expert(64)
        fw.barrier()
        with fw.phase():
            g2b = fw.sb([128, D], F32, "g2b")
            fnb = fw.sb([128, D], F32, "fnb")
            t_gb2 = Tok()
            fw.dma("sp", g2b[:], mod_d[0:1, 5 * D:6 * D].to_broadcast([128, D]), writes=[t_gb2])
            fw.dma("sp", fnb[:], fnw.to_broadcast([128, D]), writes=[t_gb2], acc=True)
            xf = [fw.sb([128, D], F32, f"xf{i}") for i in range(2)]
            t_xf = [Tok(), Tok()]
            Am = [fw.sb([128, D], F32, f"Am{i}") for i in range(2)]
            t_Am = [Tok(), Tok()]
            Gb = [fw.sb([128, D], F32, f"Gb{i}") for i in range(3)]
            t_Gb = [Tok() for _ in range(3)]
            gbi = [0]
            junk3 = fw.sb([128, D], BF16, "junk3")
            st3 = fw.sb([128, 4], F32, "st3")
            t_st3 = Tok()
            for ti in range(8):
                def fin(ti=ti):
                    i = ti % 2
                    fw.dma("sp", xf[i][:], x1_d[ti * 128:(ti + 1) * 128, :], writes=[t_xf[i]])
                    fw.dma("sp", Am[i][:], ysh_d[ti * 128:(ti + 1) * 128, :], writes=[t_Am[i]])
                    for k in range(8):
                        def comb(k=k):
                            g = gbi[0]
                            gbi[0] = (g + 1) % 3
                            fw.idma(Gb[g][:], None, yg_d, bass.IndirectOffsetOnAxis(ap=IDX[:, ti, k:k + 1], axis=0), reads=[], writes=[t_Gb[g]])
                            fw.op("dve", lambda e: e.scalar_tensor_tensor(out=Am[i][:], in0=Gb[g][:], scalar=WK[:, ti, k:k + 1], in1=Am[i][:],
                                                                          op0=ALU.mult, op1=ALU.add), reads=[t_Gb[g], t_Am[i]], writes=[t_Am[i]])
                        comb()
                    if "moe" in debug:
                        dd = dout(f"dbg_moe{ti}", [128, D])
                        t = Tok()
                        fw.dma("sp", dd, Am[i][:], reads=[t_Am[i]], writes=[t])
                        final.append(t)
                    fw.op("dve", lambda e: e.tensor_tensor(out=Am[i][:], in0=Am[i][:], in1=g2b[:], op=ALU.mult), reads=[t_gb2, t_Am[i]], writes=[t_Am[i]])
                    fw.op("dve", lambda e: e.tensor_tensor(out=xf[i][:], in0=xf[i][:], in1=Am[i][:], op=ALU.add),
                          reads=[t_Am[i], t_xf[i]], writes=[t_xf[i]])
                    fw.op("act", lambda e: e.activation(out=junk3[:], in_=xf[i][:], func=AF.Square, accum_out=st3[:, 0:1]),
                          reads=[t_xf[i], t_st3], writes=[t_st3])
                    fw.op("act", lambda e: e.activation(out=st3[:, 1:2], in_=st3[:, 0:1], func=AF.Sqrt, scale=1.0 / D, bias=EPS),
                          reads=[t_st3], writes=[t_st3])
                    fw.op("dve", lambda e: e.reciprocal(out=st3[:, 2:3], in_=st3[:, 1:2]), reads=[t_st3], writes=[t_st3])
                    fw.op("dve", lambda e: e.scalar_tensor_tensor(out=xf[i][:], in0=xf[i][:], scalar=st3[:, 2:3], in1=fnb[:], op0=ALU.mult, op1=ALU.mult),
                          reads=[t_st3, t_xf[i], t_gb2], writes=[t_xf[i]])
                    t = Tok()
                    fw.dma("act", out[ti * 128:(ti + 1) * 128, :], xf[i][:], reads=[t_xf[i]], writes=[t])
                    final.append(t)
                fin()

    fw.finish(final)


def _prep_core(inputs, b, j):
    rev = (j == 1)
    xb = inputs["x"][b]
    cb = inputs["ctx"][b]
    if rev:
        xb = xb[::-1]
        cb = cb[::-1]
    dA = 1 if rev else 0
    dB = 1 - dA
    cc = np.stack([inputs["c"][b], inputs["c_ctx"]], axis=-1)
    w_in = inputs["w_in"][0]
    m = {
        "x_own": np.ascontiguousarray(xb[0:NOWN]),
        "x_oth": np.ascontiguousarray(xb[NOWN:2 * NOWN]),
        "x_ctx": np.ascontiguousarray(cb),
        "cT": np.ascontiguousarray(cc.reshape(KT, 128, 2).transpose(1, 0, 2)),
        "ada_w": inputs["ada_w"][0],
        "ada_b": inputs["ada_b"][0][None, :],
        "n1w": np.ascontiguousarray(inputs["norm1_w"][0].reshape(KT, 128).T),
        "w_in": w_in,
        "w_gk": np.ascontiguousarray(np.concatenate([w_in[:, 4096 + 16 * dA:4096 + 16 * dA + 16],
                                                     w_in[:, 4096 + 16 * dB:4096 + 16 * dB + 16],
                                                     np.zeros((D, 96), np.float32)], axis=1)),
        "gk_up": np.ascontiguousarray(inputs["gla_gk_up"][0][[dA, dB]]),
        "gk_b": np.ascontiguousarray(inputs["gla_gk_b"][0][[dA, dB]][:, None, :]),
        "gla_nw": np.ascontiguousarray(inputs["gla_norm_w"][0].reshape(2, 128).T),
        "s5_l": np.ascontiguousarray(np.stack([np.stack([inputs["s5_lam_re"][0][d].T, inputs["s5_lam_im"][0][d].T,
                                                          np.broadcast_to(inputs["s5_log_dt"][0][d][None, :], (64, 64))]) for d in (dA, dB)])),
        "s5_bc": np.ascontiguousarray(np.stack([np.stack([inputs["s5_b_re"][0][d].transpose(1, 0, 2).reshape(64, 1024),
                                                           inputs["s5_b_im"][0][d].transpose(1, 0, 2).reshape(64, 1024),
                                                           inputs["s5_c_re"][0][d].transpose(2, 0, 1).reshape(64, 1024),
                                                           inputs["s5_c_im"][0][d].transpose(2, 0, 1).reshape(64, 1024)]) for d in (dA, dB)])),
        "s5_dv": np.ascontiguousarray(np.tile(inputs["s5_d"][0].reshape(64, 16).T, (8, 1))),
        "glu_w": inputs["s5_glu_w"][0],
        "w_s5p": inputs["w_s5_proj"][0],
        "w_glap": inputs["w_gla_proj"][0],
        "w_out": inputs["w_out"][0],
        "n2w": np.ascontiguousarray(inputs["norm2_w"][0].reshape(KT, 128).T),
        "n2row": inputs["norm2_w"][0][None, :],
        "router_w": inputs["router_w"][0],
        "router_b": inputs["router_bias"][0][None, :],
        "exp_g": inputs["exp_w_gate"][0],
        "exp_u": inputs["exp_w_up"][0],
        "exp_d": inputs["exp_w_down"][0],
        "sh_g": inputs["sh_w_gate"][0],
        "sh_u": inputs["sh_w_up"][0],
        "sh_d": inputs["sh_w_down"][0],
        "fnw": inputs["final_norm_w"][None, :],
    }
    return m


_NC_CACHE = {}


def run(inputs, debug=()):
    inputs = {k: np.asarray(v) for k, v in inputs.items()}
    key = tuple(sorted(debug))
    if key not in _NC_CACHE:
        _NC_CACHE[key] = build(debug)
    nc = _NC_CACHE[key]
    in_maps = []
    for core in range(8):
        b, j = divmod(core, 2)
        in_maps.append(_prep_core(inputs, b, j))
    res = run_bass_kernel_spmd(nc, in_maps, core_ids=list(range(8)))
    return res.results


def kernel(**inputs):
    results = run(inputs)
    outp = np.zeros((4, 2048, D), np.float32)
    for core in range(8):
        b, j = divmod(core, 2)
        o = np.asarray(results[core]["out"])
        if j == 0:
            outp[b, 0:NOWN] = o
        else:
            outp[b, NOWN:] = o[::-1]
    return outp
```
